# Optimizing a Trainium2 kernel written in Bass

```python
import math
import jax, jax.numpy as jnp
from jax import lax
import numpy as np

D_MODEL = 2048
BATCH = 4
SEQ = 2048
DEPTH = 1

HEAD_DIM = 128
SB_WIDTH = D_MODEL // 2
N_SB_HEADS = SB_WIDTH // HEAD_DIM
POOL_WIDTH = D_MODEL - SB_WIDTH
POOL_WINDOWS = (2, 4, 8, 16)
N_POOL_GROUPS = len(POOL_WINDOWS)
POOL_GROUP_DIM = POOL_WIDTH // N_POOL_GROUPS
MIX_WIDTH = SB_WIDTH + POOL_WIDTH
IN_PROJ_WIDTH = 3 * SB_WIDTH + POOL_WIDTH
N_EXPERTS = 32
TOP_K = 4
D_FF = D_MODEL
SWIGLU_ALPHA = 1.702
SWIGLU_LIMIT = 7.0
Q_BLOCK = 128
EXPERT_BLOCK = 256
EPS = 1e-6

kernel_name = "hybrid_stickbreak_pool_moe_adaln"


def rms_norm(x, w):
    xf = x.astype(jnp.float32)
    y = xf * lax.rsqrt(jnp.mean(xf * xf, axis=-1, keepdims=True) + EPS)
    return (y * w.astype(jnp.float32)).astype(x.dtype)


def stick_breaking_attention(q, k, v):
    B, S, H, d = q.shape
    nb = S // Q_BLOCK
    qh = q.astype(jnp.float32).transpose(0, 2, 1, 3)
    kh = k.astype(jnp.float32).transpose(0, 2, 1, 3)
    vh = v.astype(jnp.float32).transpose(0, 2, 1, 3)
    q_blocks = qh.reshape(B, H, nb, Q_BLOCK, d).transpose(2, 0, 1, 3, 4)
    starts = jnp.arange(nb, dtype=jnp.int32) * Q_BLOCK
    s_idx = jnp.arange(S, dtype=jnp.int32)
    inv_sqrt_d = 1.0 / math.sqrt(d)

    def one_block(args):
        qblk, start = args
        z = jnp.einsum('bhqd,bhkd->bhqk', qblk, kh) * inv_sqrt_d
        t_idx = start + jnp.arange(Q_BLOCK, dtype=jnp.int32)
        causal = s_idx[None, :] < t_idx[:, None]
        log_1m_beta = jnp.where(causal, jax.nn.log_sigmoid(-z), 0.0)
        later = lax.cumsum(log_1m_beta, axis=3, reverse=True) - log_1m_beta
        log_a = jax.nn.log_sigmoid(z) + later
        a = jnp.where(causal, jnp.exp(log_a), 0.0)
        return jnp.einsum('bhqk,bhkd->bhqd', a, vh)

    o = lax.map(one_block, (q_blocks, starts))
    return o.transpose(1, 0, 3, 2, 4).reshape(B, S, H * d)


def pool_mixer(u, w_pool, pool_scale):
    B, S, _ = u.shape
    uf = u.astype(jnp.float32)
    cs = jnp.cumsum(uf, axis=1)
    t = jnp.arange(S, dtype=jnp.int32)
    outs = []
    for g, w in enumerate(POOL_WINDOWS):
        lo, hi = g * POOL_GROUP_DIM, (g + 1) * POOL_GROUP_DIM
        cs_g = cs[..., lo:hi]
        prev = jnp.pad(cs_g, ((0, 0), (w, 0), (0, 0)))[:, :S]
        cnt = jnp.minimum(t + 1, w).astype(jnp.float32)[None, :, None]
        outs.append((cs_g - prev) / cnt - uf[..., lo:hi])
    p = jnp.stack(outs, axis=2)
    y = jnp.einsum('bsgc,gcd->bsgd', p, w_pool.astype(jnp.float32)).reshape(B, S, POOL_WIDTH)
    return (y * pool_scale.astype(jnp.float32)).astype(u.dtype)


def clamped_swiglu_expert(xb, w_in_e, b_in_e, w_out_e, b_out_e):
    gu = xb @ w_in_e + b_in_e
    g, lin = gu[:, :D_FF], gu[:, D_FF:]
    g = jnp.minimum(g, SWIGLU_LIMIT)
    lin = jnp.clip(lin, -SWIGLU_LIMIT, SWIGLU_LIMIT)
    act = g * jax.nn.sigmoid(SWIGLU_ALPHA * g) * (lin + 1.0)
    return act @ w_out_e + b_out_e


def moe_ffn(h, w_router, b_router, w_exp_in, b_exp_in, w_exp_out, b_exp_out):
    B, S, D = h.shape
    T = B * S
    N = T * TOP_K
    xf = h.reshape(T, D)
    logits = (xf @ w_router + b_router).astype(jnp.float32)
    top_vals, top_idx = lax.top_k(logits, TOP_K)
    gates = jax.nn.softmax(top_vals, axis=-1)

    eid = top_idx.reshape(N).astype(jnp.int32)
    tok = jnp.repeat(jnp.arange(T, dtype=jnp.int32), TOP_K)
    order = jnp.argsort(eid)
    eid_s, tok_s = eid[order], tok[order]
    counts = jnp.bincount(eid, length=N_EXPERTS).astype(jnp.int32)
    starts = jnp.cumsum(counts) - counts
    padded = ((counts + EXPERT_BLOCK - 1) // EXPERT_BLOCK) * EXPERT_BLOCK
    pends = jnp.cumsum(padded)
    pstarts = pends - padded
    dest_s = pstarts[eid_s] + (jnp.arange(N, dtype=jnp.int32) - starts[eid_s])

    n_blocks = (N + N_EXPERTS * (EXPERT_BLOCK - 1)) // EXPERT_BLOCK
    P = n_blocks * EXPERT_BLOCK
    tok_buf = jnp.full((P,), T, dtype=jnp.int32).at[dest_s].set(tok_s)
    x_ext = jnp.concatenate([xf, jnp.zeros((1, D), xf.dtype)], axis=0)
    x_pad = x_ext[tok_buf].reshape(n_blocks, EXPERT_BLOCK, D)
    block_start = jnp.arange(n_blocks, dtype=jnp.int32) * EXPERT_BLOCK
    block_e = jnp.minimum(jnp.searchsorted(pends, block_start, side='right'), N_EXPERTS - 1)

    def run_block(args):
        xb, e = args
        return clamped_swiglu_expert(xb, w_exp_in[e], b_exp_in[e], w_exp_out[e], b_exp_out[e])

    y_pad = lax.map(run_block, (x_pad, block_e)).reshape(P, D)
    dest = jnp.zeros((N,), jnp.int32).at[order].set(dest_s)
    y = jnp.sum(y_pad[dest].reshape(T, TOP_K, D).astype(jnp.float32) * gates[..., None], axis=1)
    return y.reshape(B, S, D).astype(h.dtype)


def setup_inputs(seed: int = 0) -> dict:
    key = jax.random.key(seed)
    ks = jax.random.split(key, 18)
    L = DEPTH

    def nrm(k, shape, scale):
        return jax.random.normal(k, shape, jnp.float32) * scale

    return {
        "x": nrm(ks[0], (BATCH, SEQ, D_MODEL), 1.0),
        "c": nrm(ks[1], (BATCH, D_MODEL), 1.0),
        "norm1_w": 1.0 + nrm(ks[2], (L, D_MODEL), 0.05),
        "norm2_w": 1.0 + nrm(ks[3], (L, D_MODEL), 0.05),
        "w_ada": nrm(ks[4], (L, D_MODEL, 6 * D_MODEL), 0.3 * D_MODEL ** -0.5),
        "b_ada": nrm(ks[5], (L, 6 * D_MODEL), 0.02),
        "w_in": nrm(ks[6], (L, D_MODEL, IN_PROJ_WIDTH), D_MODEL ** -0.5),
        "q_norm_w": 1.0 + nrm(ks[7], (L, HEAD_DIM), 0.05),
        "k_norm_w": 1.0 + nrm(ks[8], (L, HEAD_DIM), 0.05),
        "w_pool": nrm(ks[9], (L, N_POOL_GROUPS, POOL_GROUP_DIM, POOL_GROUP_DIM), POOL_GROUP_DIM ** -0.5),
        "pool_scale": 1.0 + nrm(ks[10], (L, POOL_WIDTH), 0.1),
        "w_o": nrm(ks[11], (L, MIX_WIDTH, D_MODEL), MIX_WIDTH ** -0.5),
        "w_router": nrm(ks[12], (L, D_MODEL, N_EXPERTS), D_MODEL ** -0.5),
        "b_router": nrm(ks[13], (L, N_EXPERTS), 0.01),
        "w_exp_in": nrm(ks[14], (L, N_EXPERTS, D_MODEL, 2 * D_FF), D_MODEL ** -0.5),
        "b_exp_in": nrm(ks[15], (L, N_EXPERTS, 2 * D_FF), 0.02),
        "w_exp_out": nrm(ks[16], (L, N_EXPERTS, D_FF, D_MODEL), D_FF ** -0.5),
        "b_exp_out": nrm(ks[17], (L, N_EXPERTS, D_MODEL), 0.02),
    }


def reference(x, c, norm1_w, norm2_w, w_ada, b_ada, w_in, q_norm_w, k_norm_w, w_pool,
              pool_scale, w_o, w_router, b_router, w_exp_in, b_exp_in, w_exp_out, b_exp_out):
    B, S, D = x.shape
    c_act = jax.nn.silu(c)
    for l in range(DEPTH):
        mod = c_act @ w_ada[l] + b_ada[l]
        shift1, scale1, gate1, shift2, scale2, gate2 = [m[:, None, :] for m in jnp.split(mod, 6, axis=-1)]

        h = rms_norm(x, norm1_w[l]) * (1.0 + scale1) + shift1
        proj = h @ w_in[l]
        q = proj[..., :SB_WIDTH].reshape(B, S, N_SB_HEADS, HEAD_DIM)
        k = proj[..., SB_WIDTH:2 * SB_WIDTH].reshape(B, S, N_SB_HEADS, HEAD_DIM)
        v = proj[..., 2 * SB_WIDTH:3 * SB_WIDTH].reshape(B, S, N_SB_HEADS, HEAD_DIM)
        u = proj[..., 3 * SB_WIDTH:]
        q = rms_norm(q, q_norm_w[l])
        k = rms_norm(k, k_norm_w[l])
        o_sb = stick_breaking_attention(q, k, v).astype(x.dtype)
        o_pool = pool_mixer(u, w_pool[l], pool_scale[l])
        mixed = jnp.concatenate([o_sb, o_pool], axis=-1) @ w_o[l]
        x = x + gate1 * mixed

        h2 = rms_norm(x, norm2_w[l]) * (1.0 + scale2) + shift2
        y = moe_ffn(h2, w_router[l], b_router[l], w_exp_in[l], b_exp_in[l], w_exp_out[l], b_exp_out[l])
        x = x + gate2 * y
    return x
```

```python
import contextlib
import numpy as np
import concourse.bass as bass
import concourse.mybir as mybir
from concourse.bass_utils import run_bass_kernel_spmd

F32 = mybir.dt.float32
BF16 = mybir.dt.bfloat16
I32 = mybir.dt.int32
AF = mybir.ActivationFunctionType
ALU = mybir.AluOpType

D = 2048
NT = 1024
NE = 32
CAP = 352
EPS = 1e-6
NEGB = -30000.0


class Tl:
    __slots__ = ("ap", "w", "r", "dsem", "dcnt", "name")

    def __init__(s, ap, name=""):
        s.ap = ap; s.w = None; s.r = {}; s.dsem = None; s.dcnt = 0; s.name = name


class Eng:
    def __init__(s, k, name, eng, is_pe=False):
        s.eng = eng; s.sem = k.newsem(name); s.n = 0; s.waited = {}; s.is_pe = is_pe; s.name = name


class KB:
    def __init__(s, nc, es):
        s.nc = nc; s.es = es; s.nsem = 0; s.dsems = []
        s.pe = Eng(s, "pe", nc.tensor, True)
        s.act = Eng(s, "act", nc.scalar)
        s.dve = Eng(s, "dve", nc.vector)
        s.pool = Eng(s, "pool", nc.gpsimd)
        s.sp = Eng(s, "sp", nc.sync)
        s.engs = [s.pe, s.act, s.dve, s.pool, s.sp]

    def newsem(s, name):
        s.nsem += 1
        return s.es.enter_context(s.nc.semaphore(f"{name}_{s.nsem}"))

    def wait(s, e, tok):
        sem, val, src = tok
        if src is e:
            if e.is_pe or e.n - val >= 2:
                return
        key = id(sem)
        if e.waited.get(key, 0) >= val:
            return
        e.eng.wait_ge(sem, val)
        e.waited[key] = val

    def deps(s, e, reads, writes, dma_sem=None):
        for t in reads:
            if t.w is not None:
                s.wait(e, t.w)
        for t in writes:
            if t.w is not None and not (dma_sem is not None and t.w[0] is dma_sem):
                s.wait(e, t.w)
            for tok in t.r.values():
                s.wait(e, tok)

    def finish(s, tok, reads, writes):
        for t in writes:
            t.w = tok; t.r = {}
        for t in reads:
            key = id(tok[0]); old = t.r.get(key)
            if old is None or old[1] < tok[1]:
                t.r[key] = tok

    def op(s, e, fn, reads=(), writes=()):
        s.deps(e, reads, writes)
        ins = fn()
        e.n += 1
        ins.then_inc(e.sem, 1)
        tok = (e.sem, e.n, e)
        s.finish(tok, reads, writes)
        return tok

    def dma(s, e, out, in_, reads=(), writes=()):
        tgt = writes[0]
        if tgt.dsem is None:
            tgt.dsem = s.newsem("d" + tgt.name); s.dsems.append(tgt)
        s.deps(e, reads, writes, dma_sem=tgt.dsem)
        ins = e.eng.dma_start(out=out, in_=in_)
        tgt.dcnt += 16
        ins.then_inc(tgt.dsem, 16)
        tok = (tgt.dsem, tgt.dcnt, None)
        s.finish(tok, reads, writes)
        return tok

    def mmg(s, out_t, out_ap, ops, reads):
        nc = s.nc
        s.deps(s.pe, reads, [out_t])
        n = len(ops)
        ins = None
        for i, (l, r) in enumerate(ops):
            ins = nc.tensor.matmul(out=out_ap, lhsT=l, rhs=r, start=(i == 0), stop=(i == n - 1))
        s.pe.n += 1
        ins.then_inc(s.pe.sem, 1)
        tok = (s.pe.sem, s.pe.n, s.pe)
        s.finish(tok, reads, [out_t])
        return tok

    def barrier(s):
        for e in s.engs:
            for f in s.engs:
                if f is not e and f.n > 0:
                    s.wait(e, (f.sem, f.n, f))
            for t in s.dsems:
                if t.dcnt:
                    s.wait(e, (t.dsem, t.dcnt, None))


def bcast_rows(ap1d_tensor, offset, n, parts=128):
    return bass.AP(ap1d_tensor, offset, [[0, parts], [1, n]])


def build_nc(ne_w=NE, DEBUG=False):
    N_EXPERTS_RUN = ne_w
    nc = bass.Bass("TRN2", target_bir_lowering=False)
    dt = nc.dram_tensor
    x_own = dt("x_own", [NT, D], F32, kind="ExternalInput").ap()
    x_prev = dt("x_prev", [NT, D], F32, kind="ExternalInput").ap()
    c_own = dt("c_own", [128, 16], F32, kind="ExternalInput").ap()
    flags = dt("flags", [128, 2], F32, kind="ExternalInput").ap()
    norm1_w = dt("norm1_w", [128, 16], F32, kind="ExternalInput").ap()
    norm2_w = dt("norm2_w", [D], F32, kind="ExternalInput").ap()
    w_ada = dt("w_ada", [D, 6 * D], F32, kind="ExternalInput").ap()
    b_ada = dt("b_ada", [6 * D], F32, kind="ExternalInput").ap()
    w_in = dt("w_in", [D, 2 * D], F32, kind="ExternalInput").ap()
    qk_w = dt("qk_w", [128, 2], F32, kind="ExternalInput").ap()
    w_pool = dt("w_pool", [4, 256, 256], F32, kind="ExternalInput").ap()
    pool_scale = dt("pool_scale", [128, 8], F32, kind="ExternalInput").ap()
    w_o = dt("w_o", [D, D], F32, kind="ExternalInput").ap()
    w_router = dt("w_router", [D, NE], F32, kind="ExternalInput").ap()
    b_router = dt("b_router", [NE], F32, kind="ExternalInput").ap()
    w_exp_in = dt("w_exp_in", [ne_w, D, 2 * D], F32, kind="ExternalInput").ap()
    b_exp_in = dt("b_exp_in", [NE * 32, 128], F32, kind="ExternalInput").ap()
    w_exp_out = dt("w_exp_out", [ne_w, D, D], F32, kind="ExternalInput").ap()
    b_exp_out = dt("b_exp_out", [NE, D], F32, kind="ExternalInput").ap()
    out = dt("out", [NT, D], F32, kind="ExternalOutput").ap()
    mod_d = dt("mod_d", [6 * D], F32, kind="Internal").ap()
    x1_d = dt("x1_d", [NT, D], F32, kind="ExternalOutput" if DEBUG else "Internal").ap()

    with contextlib.ExitStack() as es:
        k = KB(nc, es)
        PE, ACT, DVE, POOL, SP = k.pe, k.act, k.dve, k.pool, k.sp

        def sb(name, shape, dtype):
            return es.enter_context(nc.sbuf_tensor(name, shape, dtype))

        wbuf = [Tl(sb(f"wbuf{i}", [128, 16, 512], BF16), f"wbuf{i}") for i in range(3)]
        big = sb("big", [128, 24576], F32)
        ident_f = Tl(sb("ident_f", [128, 128], F32), "identf")
        ident_b = Tl(sb("ident_b", [128, 128], BF16), "identb")
        tri_b = Tl(sb("tri_b", [128, 128], BF16), "tri")
        ones_b = Tl(sb("ones_b", [128, 128], BF16), "ones")
        strict_b = Tl(sb("strict_b", [128, 128], BF16), "strict")
        iota_c = Tl(sb("iota_c", [128, CAP], F32), "iotac")
        small = Tl(sb("small", [128, 64], F32), "small")
        modT = Tl(sb("modT", [128, 96], F32), "modT")
        flg = Tl(sb("flg", [128, 2], F32), "flg")
        ps = [Tl(es.enter_context(nc.psum_tensor(f"ps{i}", [128, 512], F32)), f"ps{i}") for i in range(8)]
        mod_dT = Tl(mod_d, "mod_d")
        x1_dT = Tl(x1_d, "x1_d")
        outT = Tl(out, "out")

        ZC = small.ap[:, 0:1]

        with contextlib.ExitStack() as cs:
            ii = Tl(cs.enter_context(nc.sbuf_tensor("ii", [128, 512], I32)), "ii")
            ff = Tl(cs.enter_context(nc.sbuf_tensor("ff", [128, 512], F32)), "ff")
            k.op(POOL, lambda: nc.gpsimd.iota(ii.ap[:, 0:128], pattern=[[1, 128]], base=0, channel_multiplier=-1), writes=[ii])
            k.op(DVE, lambda: nc.vector.tensor_copy(out=ff.ap[:, 0:128], in_=ii.ap[:, 0:128]), reads=[ii], writes=[ff])
            k.op(DVE, lambda: nc.vector.tensor_scalar(out=ident_f.ap[:], in0=ff.ap[:, 0:128], scalar1=0.0, scalar2=None, op0=ALU.is_equal), reads=[ff], writes=[ident_f])
            k.op(DVE, lambda: nc.vector.tensor_scalar(out=ident_b.ap[:], in0=ff.ap[:, 0:128], scalar1=0.0, scalar2=None, op0=ALU.is_equal), reads=[ff], writes=[ident_b])
            k.op(DVE, lambda: nc.vector.tensor_scalar(out=tri_b.ap[:], in0=ff.ap[:, 0:128], scalar1=0.0, scalar2=None, op0=ALU.is_le), reads=[ff], writes=[tri_b])
            k.op(DVE, lambda: nc.vector.tensor_scalar(out=strict_b.ap[:], in0=ff.ap[:, 0:128], scalar1=0.0, scalar2=None, op0=ALU.is_gt), reads=[ff], writes=[strict_b])
            k.op(DVE, lambda: nc.vector.memset(ones_b.ap[:], 1.0), writes=[ones_b])
            k.op(DVE, lambda: nc.vector.memset(small.ap[:], 0.0), writes=[small])
            k.op(POOL, lambda: nc.gpsimd.iota(ii.ap[:, 0:CAP], pattern=[[1, CAP]], base=0, channel_multiplier=0), reads=[], writes=[ii])
            k.op(DVE, lambda: nc.vector.tensor_copy(out=iota_c.ap[:], in_=ii.ap[:, 0:CAP]), reads=[ii], writes=[iota_c])
            k.dma(SP, flg.ap[:], flags[:, :], writes=[flg])
            k.barrier()

        cTb = Tl(sb("cTb", [128, 16], BF16), "cTb")
        stg = Tl(sb("stg", [1, 512], F32), "stg")
        bst = Tl(sb("bst", [1, 512], F32), "bst")
        wav = w_ada.rearrange("(c p) f -> p c f", p=128)

        def mod_load(nb, wb):
            k.dma(POOL, wb.ap[:], wav[:, :, nb * 512:(nb + 1) * 512], writes=[wb])

        def mod_block(nb, wb, pst, load=True):
            k.dma(SP, bst.ap[:], bass.AP(b_ada.tensor, nb * 512, [[0, 1], [1, 512]]), writes=[bst])
            if load:
                mod_load(nb, wb)
            k.mmg(pst, pst.ap[0:1, :], [(cTb.ap[:, kc:kc + 1], wb.ap[:, kc, :]) for kc in range(16)], reads=[cTb, wb])
            k.op(DVE, lambda: nc.vector.tensor_tensor(out=stg.ap[:], in0=pst.ap[0:1, :], in1=bst.ap[:], op=ALU.add), reads=[pst, bst], writes=[stg])
            k.dma(SP, bass.AP(mod_d.tensor, nb * 512, [[0, 1], [1, 512]]), stg.ap[:], reads=[stg], writes=[mod_dT])

        with contextlib.ExitStack() as cs:
            cT = Tl(cs.enter_context(nc.sbuf_tensor("cT", [128, 16], F32)), "cT")
            k.dma(SP, cT.ap[:], c_own[:, :], writes=[cT])
            k.op(ACT, lambda: nc.scalar.activation(out=cTb.ap[:], in_=cT.ap[:], func=AF.Silu), reads=[cT], writes=[cTb])
            for nb in range(8):
                mod_block(nb, wbuf[nb % 3], ps[nb % 2])
            with nc.allow_non_contiguous_dma(reason="small one-time relayout of the modulation vector"):
                k.dma(SP, modT.ap[:, 0:32], bass.AP(mod_d.tensor, 0, [[1, 128], [128, 32]]), reads=[mod_dT], writes=[modT])
            k.barrier()

        A1T = Tl(sb("A1T", [128, 16], F32), "A1T")
        n1w = Tl(sb("n1w", [128, 16], F32), "n1w")
        k.dma(SP, n1w.ap[:], norm1_w[:, :], writes=[n1w])
        k.op(DVE, lambda: nc.vector.scalar_tensor_tensor(out=A1T.ap[:], in0=modT.ap[:, 16:32], scalar=1.0, in1=n1w.ap[:], op0=ALU.add, op1=ALU.mult), reads=[modT, n1w], writes=[A1T])
        qkw = Tl(sb("qkw", [128, 2], F32), "qkw")
        k.dma(SP, qkw.ap[:], qk_w[:, :], writes=[qkw])
        k.op(DVE, lambda: nc.vector.tensor_scalar(out=small.ap[:, 2:3], in0=qkw.ap[:, 0:1], scalar1=float(128 ** -0.5), scalar2=None, op0=ALU.mult), reads=[qkw], writes=[small])
        k.op(DVE, lambda: nc.vector.tensor_copy(out=small.ap[:, 3:4], in_=qkw.ap[:, 1:2]), reads=[qkw], writes=[small])

        hT_ap = big[:, 0:16384].bitcast(BF16).rearrange("p (c t) -> p c t", c=16)
        hT = Tl(hT_ap, "hT")
        spare = big[:, 16384:24576]

        def rms_rstd(src_t, src_ap, junk_ap, ss_t, col, n):
            k.op(DVE, lambda: nc.vector.memset(ss_t.ap[:, col:col + 1], 0.0), writes=[ss_t])
            k.op(ACT, lambda: nc.scalar.activation(out=junk_ap, in_=src_ap, func=AF.Square, accum_out=ss_t.ap[:, col:col + 1]), reads=[src_t, ss_t], writes=[ss_t])
            k.op(DVE, lambda: nc.vector.tensor_scalar(out=ss_t.ap[:, col + 1:col + 2], in0=ss_t.ap[:, col:col + 1], scalar1=1.0 / n, scalar2=EPS, op0=ALU.mult, op1=ALU.add), reads=[ss_t], writes=[ss_t])
            k.op(ACT, lambda: nc.scalar.activation(out=ss_t.ap[:, col + 1:col + 2], in_=ss_t.ap[:, col + 1:col + 2], func=AF.Sqrt), reads=[ss_t], writes=[ss_t])
            k.op(DVE, lambda: nc.vector.reciprocal(out=ss_t.ap[:, col + 2:col + 3], in_=ss_t.ap[:, col + 1:col + 2]), reads=[ss_t], writes=[ss_t])

        concat_cm = nc.sbuf_tensor("concatT", [128, 16, NT], BF16)
        concatT = Tl(concat_cm.__enter__(), "concatT")

        with contextlib.ExitStack() as cs:
            xts = [Tl(spare[:, i * 2048:(i + 1) * 2048], f"xt{i}") for i in range(2)]
            junk = Tl(spare[:, 4096:5120].bitcast(BF16), "junk")
            sst = [Tl(cs.enter_context(nc.sbuf_tensor(f"ssB{i}", [128, 4], F32)), f"ssB{i}") for i in range(2)]
            for ti in range(16):
                src = x_prev if ti < 8 else x_own
                r0 = (ti % 8) * 128
                xt = xts[ti % 2]; ss = sst[ti % 2]
                k.dma(SP, xt.ap, src[r0:r0 + 128, :], writes=[xt])
                rms_rstd(xt, xt.ap, junk.ap, ss, 0, D)
                k.op(DVE, lambda: nc.vector.tensor_scalar(out=xt.ap, in0=xt.ap, scalar1=ss.ap[:, 2:3], scalar2=None, op0=ALU.mult), reads=[xt, ss], writes=[xt])
                for g in range(4):
                    pst = ps[g]
                    k.deps(PE, [xt, ident_f], [pst])
                    ins = None
                    for j in range(4):
                        c = 4 * g + j
                        ins = nc.tensor.transpose(out=pst.ap[:, j * 128:(j + 1) * 128], in_=xt.ap[:, c * 128:(c + 1) * 128], identity=ident_f.ap[:])
                    PE.n += 1; ins.then_inc(PE.sem, 1); tok = (PE.sem, PE.n, PE); k.finish(tok, [xt, ident_f], [pst])
                    for j in range(4):
                        c = 4 * g + j
                        if j % 2 == 0:
                            k.op(ACT, lambda: nc.scalar.activation(out=hT.ap[:, c, ti * 128:(ti + 1) * 128], in_=pst.ap[:, j * 128:(j + 1) * 128], func=AF.Identity, scale=A1T.ap[:, c:c + 1], bias=modT.ap[:, c:c + 1]), reads=[pst, A1T, modT], writes=[hT])
                        else:
                            k.op(DVE, lambda: nc.vector.tensor_scalar(out=hT.ap[:, c, ti * 128:(ti + 1) * 128], in0=pst.ap[:, j * 128:(j + 1) * 128], scalar1=A1T.ap[:, c:c + 1], scalar2=modT.ap[:, c:c + 1], op0=ALU.mult, op1=ALU.add), reads=[pst, A1T, modT], writes=[hT])
            k.barrier()

        w_in_v = w_in.rearrange("(c p) f -> p c f", p=128)
        with contextlib.ExitStack() as cs:
            def sbc(name, shape, dtype):
                return Tl(cs.enter_context(nc.sbuf_tensor(name, shape, dtype)), name)
            NEG = Tl(spare[:, 0:2048].rearrange("p (j t) -> p j t", j=4), "NEG")
            QT = [Tl(spare[:, 2048 + i * 512:2048 + (i + 1) * 512].bitcast(BF16), f"QT{i}") for i in range(2)]
            KT = [Tl(spare[:, 3072 + i * 1024:3072 + (i + 1) * 1024].bitcast(BF16), f"KT{i}") for i in range(2)]
            VV = [Tl(spare[:, 5120 + i * 1024:5120 + (i + 1) * 1024].bitcast(BF16).rearrange("p (t d) -> p t d", t=16), f"V{i}") for i in range(2)]
            sq = [sbc("sq0", [128, 512], BF16)] * 2
            rstd = [sbc("rstd0", [128, 512], F32)] * 2
            vtmp = sbc("vtmp", [128, 512], BF16)
            Eb = [sbc(f"E{i}", [128, 512], F32) for i in range(2)]
            Lb = [sbc(f"L{i}", [128, 512], BF16) for i in range(2)]
            T1 = sbc("T1", [128, 512], F32)
            Zmb = [sbc(f"Zm{i}", [128, 512], F32) for i in range(2)]
            T2 = [sbc(f"T2{i}", [128, 512], F32) for i in range(2)]
            Ab = [sbc(f"A{i}", [128, 512], BF16) for i in range(2)]
            Rb = sbc("Rb", [128, 512], F32)
            ps2b = ps[2].ap[:].bitcast(BF16)
            ii = Tl(T1.ap[:].bitcast(I32), "iiC")
            for j in range(4):
                k.op(POOL, lambda: nc.gpsimd.iota(ii.ap, pattern=[[1, 512]], base=-128 * j, channel_multiplier=-1), writes=[ii])
                k.op(DVE, lambda: nc.vector.tensor_copy(out=NEG.ap[:, j, :], in_=ii.ap), reads=[ii], writes=[NEG])
                k.op(DVE, lambda: nc.vector.tensor_scalar(out=NEG.ap[:, j, :], in0=NEG.ap[:, j, :], scalar1=0.0, scalar2=NEGB, op0=ALU.is_le, op1=ALU.mult), reads=[NEG], writes=[NEG])
            k.barrier()

            def qk_norm(pst, dst_t, dst_ap, wcol):
                sqt = sq[0]; rs = rstd[0]
                k.op(ACT, lambda: nc.scalar.activation(out=sqt.ap[:], in_=pst.ap[:], func=AF.Square), reads=[pst], writes=[sqt])
                k.mmg(ps[2], ps[2].ap[:], [(ones_b.ap[:], sqt.ap[:])], reads=[ones_b, sqt])
                k.op(ACT, lambda: nc.scalar.activation(out=rs.ap[:], in_=ps[2].ap[:], func=AF.Ln, scale=1.0 / 128, bias=small.ap[:, 5:6]), reads=[ps[2], small], writes=[rs])
                k.op(ACT, lambda: nc.scalar.activation(out=rs.ap[:], in_=rs.ap[:], func=AF.Exp, scale=-0.5), reads=[rs], writes=[rs])
                k.op(DVE, lambda: nc.vector.scalar_tensor_tensor(out=dst_ap, in0=pst.ap[:], scalar=small.ap[:, wcol:wcol + 1], in1=rs.ap[:], op0=ALU.mult, op1=ALU.mult), reads=[pst, rs, small], writes=[dst_t])

            k.op(DVE, lambda: nc.vector.memset(small.ap[:, 5:6], EPS), writes=[small])
            acc_ctr = [0]

            def inproj_items(h):
                wb = wbuf[h % 2]; qt = QT[h % 2]; kt = KT[h % 2]; vt = VV[h % 2]
                items = []

                def it_w():
                    for j, c0_ in enumerate((128 * h, 1024 + 128 * h, 2048 + 128 * h)):
                        k.dma(POOL, wb.ap[:, :, j * 128:(j + 1) * 128], w_in_v[:, :, c0_:c0_ + 128], writes=[wb])
                items.append(it_w)

                def mk_q(n_):
                    def f():
                        pst = ps[acc_ctr[0] % 2]; acc_ctr[0] += 1
                        k.mmg(pst, pst.ap[:], [(wb.ap[:, kc, 0:128], hT.ap[:, kc, 1024 + n_ * 512:1024 + (n_ + 1) * 512]) for kc in range(16)], reads=[wb, hT])
                        qk_norm(pst, qt, qt.ap[:, n_ * 512:(n_ + 1) * 512], 2)
                    return f

                def mk_k(n_):
                    def f():
                        pst = ps[acc_ctr[0] % 2]; acc_ctr[0] += 1
                        k.mmg(pst, pst.ap[:], [(wb.ap[:, kc, 128:256], hT.ap[:, kc, n_ * 512:(n_ + 1) * 512]) for kc in range(16)], reads=[wb, hT])
                        qk_norm(pst, kt, kt.ap[:, n_ * 512:(n_ + 1) * 512], 3)
                    return f

                def mk_v(n_):
                    def f():
                        pst = ps[acc_ctr[0] % 2]; acc_ctr[0] += 1
                        k.mmg(pst, pst.ap[:], [(wb.ap[:, kc, 256:384], hT.ap[:, kc, n_ * 512:(n_ + 1) * 512]) for kc in range(16)], reads=[wb, hT])
                        k.op(DVE, lambda: nc.vector.tensor_copy(out=vtmp.ap[:], in_=pst.ap[:]), reads=[pst], writes=[vtmp])
                        k.deps(PE, [vtmp, ident_b], [ps[2]])
                        ins = None
                        for j in range(4):
                            ins = nc.tensor.transpose(out=ps2b[:, j * 128:(j + 1) * 128], in_=vtmp.ap[:, j * 128:(j + 1) * 128], identity=ident_b.ap[:])
                        PE.n += 1; ins.then_inc(PE.sem, 1); tok = (PE.sem, PE.n, PE); k.finish(tok, [vtmp, ident_b], [ps[2]])
                        k.op(DVE, lambda: nc.vector.tensor_copy(out=vt.ap[:, 4 * n_:4 * n_ + 4, :], in_=ps2b[:, 0:512].rearrange("p (t d) -> p t d", t=4)), reads=[ps[2]], writes=[vt])
                    return f
                items += [mk_k(0), mk_q(0), mk_k(1), mk_v(0), mk_k(2), mk_v(1), mk_q(1), mk_k(3), mk_v(2), mk_v(3)]
                return items

            mod_next = [8]

            mod_load(8, wbuf[2])

            def mod_item():
                if mod_next[0] < 24:
                    mod_block(mod_next[0], wbuf[2], ps[2], load=False); mod_next[0] += 1
                    if mod_next[0] < 24:
                        mod_load(mod_next[0], wbuf[2])

            for f in inproj_items(0):
                f()
            for h in range(8):
                qt = QT[h % 2]; kt = KT[h % 2]; vt = VV[h % 2]
                pending = inproj_items(h + 1) if h < 7 else []
                gstep = 0
                for qc in range(2):
                    steps = []
                    for j in (3, 2, 1, 0):
                        kb = 4 * qc + j
                        steps.append((1024 + kb * 128, 8 + kb, j, False))
                    for kb in range(4 * qc - 1, -1, -1):
                        steps.append((1024 + kb * 128, 8 + kb, None, False))
                    for kb in range(7, -1, -1):
                        steps.append((kb * 128, kb, None, True))
                    n = len(steps)
                    k.op(DVE, lambda: nc.vector.memset(Rb.ap[:], 0.0), writes=[Rb])
                    qap = qt.ap[:, qc * 512:(qc + 1) * 512]
                    oT = ps[7]
                    zsrc = [None] * n
                    mod_item()
                    for it in range(n + 3):
                        gstep += 1
                        if pending and (gstep == 1 or (gstep >= 6 and gstep % 2 == 0)):
                            pending.pop(0)()
                        if it < n:
                            i = it
                            kcol, vti, dj, isprev = steps[i]
                            Z = ps[3 + i % 2]; E = Eb[i % 2]; L = Lb[i % 2]
                            k.mmg(Z, Z.ap[:], [(kt.ap[:, kcol:kcol + 128], qap)], reads=[kt, qt])
                            bias_ap = flg.ap[:, 0:1] if isprev else ZC
                            if dj is not None:
                                Zm = Zmb[i % 2]
                                k.op(DVE, lambda: nc.vector.tensor_tensor(out=Zm.ap[:], in0=Z.ap[:], in1=NEG.ap[:, dj, :], op=ALU.add), reads=[Z, NEG], writes=[Zm])
                                zs = Zm
                            else:
                                zs = Z
                            zsrc[i] = zs
                            k.op(ACT, lambda: nc.scalar.activation(out=E.ap[:], in_=zs.ap[:], func=AF.Exp, bias=bias_ap, scale=1.0), reads=[zs, flg, small], writes=[E])
                            k.op(ACT, lambda: nc.scalar.activation(out=L.ap[:], in_=E.ap[:], func=AF.Ln, bias=1.0, scale=1.0), reads=[E], writes=[L])
                        if 0 <= it - 1 < n:
                            i = it - 1
                            kcol, vti, dj, isprev = steps[i]
                            E = Eb[i % 2]; L = Lb[i % 2]; A = Ab[i % 2]; T2t = T2[i % 2]
                            zs = zsrc[i]
                            bias_ap = flg.ap[:, 0:1] if isprev else ZC
                            k.mmg(ps[5], ps[5].ap[:], [(tri_b.ap[:], L.ap[:])], reads=[tri_b, L])
                            if i < n - 1:
                                k.mmg(ps[6], ps[6].ap[:], [(ones_b.ap[:], L.ap[:])], reads=[ones_b, L])
                            k.op(DVE, lambda: nc.vector.tensor_tensor(out=T1.ap[:], in0=zs.ap[:], in1=Rb.ap[:], op=ALU.subtract), reads=[zs, Rb], writes=[T1])
                            if i < n - 1:
                                k.op(DVE, lambda: nc.vector.tensor_tensor(out=Rb.ap[:], in0=Rb.ap[:], in1=ps[6].ap[:], op=ALU.add), reads=[Rb, ps[6]], writes=[Rb])
                            k.op(DVE, lambda: nc.vector.tensor_tensor(out=T2t.ap[:], in0=T1.ap[:], in1=ps[5].ap[:], op=ALU.subtract), reads=[T1, ps[5]], writes=[T2t])
                        if 0 <= it - 2 < n:
                            i = it - 2
                            kcol, vti, dj, isprev = steps[i]
                            A = Ab[i % 2]; T2t = T2[i % 2]
                            bias_ap = flg.ap[:, 0:1] if isprev else ZC
                            k.op(ACT, lambda: nc.scalar.activation(out=A.ap[:], in_=T2t.ap[:], func=AF.Exp, bias=bias_ap, scale=1.0), reads=[T2t, flg, small], writes=[A])
                        if 0 <= it - 3 < n:
                            i = it - 3
                            kcol, vti, dj, isprev = steps[i]
                            A = Ab[i % 2]
                            k.deps(PE, [vt, A], [oT])
                            ins = nc.tensor.matmul(out=oT.ap[:], lhsT=vt.ap[:, vti, :], rhs=A.ap[:], start=(i == 0), stop=(i == n - 1))
                            PE.n += 1; ins.then_inc(PE.sem, 1); tok = (PE.sem, PE.n, PE); k.finish(tok, [vt, A], [oT])
                    k.op(ACT, lambda: nc.scalar.copy(out=concatT.ap[:, h, qc * 512:(qc + 1) * 512], in_=oT.ap[:]), reads=[oT], writes=[concatT])
                while pending:
                    pending.pop(0)()
            while mod_next[0] < 24:
                mod_item()
            k.barrier()

        with contextlib.ExitStack() as cs:
            def sbc(name, shape, dtype):
                return Tl(cs.enter_context(nc.sbuf_tensor(name, shape, dtype)), name)
            UW = 16 + NT
            U = [Tl(spare[:, i * 1040:(i + 1) * 1040], f"U{i}") for i in range(2)]
            SA = Tl(spare[:, 2080:3120], "SA"); SB_ = Tl(spare[:, 3120:4160], "SB")
            pT = Tl(spare[:, 4160:6208].bitcast(BF16).rearrange("p (c t) -> p c t", c=4), "pT")
            wpool = sbc("wpool", [128, 4, 2, 256], BF16)
            pscale = sbc("pscale", [128, 8], F32)
            invc = sbc("invc", [128, 4, 16], F32)
            tmp16 = sbc("tmp16", [128, 16], F32)
            iiD = sbc("iiD", [128, 16], I32)
            k.dma(POOL, wpool.ap[:], w_pool.rearrange("g (c p) d -> p g c d", p=128), writes=[wpool])
            k.dma(SP, pscale.ap[:], pool_scale[:, :], writes=[pscale])
            k.op(POOL, lambda: nc.gpsimd.iota(iiD.ap[:], pattern=[[1, 16]], base=1, channel_multiplier=0), writes=[iiD])
            k.op(DVE, lambda: nc.vector.tensor_copy(out=tmp16.ap[:], in_=iiD.ap[:]), reads=[iiD], writes=[tmp16])
            k.op(DVE, lambda: nc.vector.tensor_scalar(out=small.ap[:, 4:5], in0=flg.ap[:, 1:2], scalar1=1024.0, scalar2=None, op0=ALU.mult), reads=[flg], writes=[small])
            k.op(DVE, lambda: nc.vector.tensor_scalar(out=tmp16.ap[:], in0=tmp16.ap[:], scalar1=small.ap[:, 4:5], scalar2=None, op0=ALU.add), reads=[small, tmp16], writes=[tmp16])
            for g in range(4):
                k.op(DVE, lambda: nc.vector.tensor_scalar(out=invc.ap[:, g, :], in0=tmp16.ap[:], scalar1=float(2 ** (g + 1)), scalar2=None, op0=ALU.min), reads=[tmp16], writes=[invc])
            k.op(DVE, lambda: nc.vector.reciprocal(out=invc.ap[:], in_=invc.ap[:]), reads=[invc], writes=[invc])
            for g in range(4):
                w = 2 ** (g + 1)
                wb = wbuf[g % 2]
                k.dma(POOL, wb.ap[:, :, 0:256], w_in_v[:, :, 3072 + 256 * g:3072 + 256 * (g + 1)], writes=[wb])
                for cc in range(2):
                    Ut = U[cc]
                    pst = ps[2]
                    k.mmg(pst, pst.ap[:, 0:128], [(wb.ap[:, kc, cc * 128:(cc + 1) * 128], hT.ap[:, kc, 896:1024]) for kc in range(16)], reads=[wb, hT])
                    k.op(DVE, lambda: nc.vector.tensor_scalar(out=Ut.ap[:, 0:16], in0=pst.ap[:, 112:128], scalar1=flg.ap[:, 1:2], scalar2=None, op0=ALU.mult), reads=[pst, flg], writes=[Ut])
                    for n_ in range(2):
                        pst = ps[n_ % 2]
                        k.mmg(pst, pst.ap[:], [(wb.ap[:, kc, cc * 128:(cc + 1) * 128], hT.ap[:, kc, 1024 + n_ * 512:1024 + (n_ + 1) * 512]) for kc in range(16)], reads=[wb, hT])
                        k.op(ACT, lambda: nc.scalar.copy(out=Ut.ap[:, 16 + n_ * 512:16 + (n_ + 1) * 512], in_=pst.ap[:]), reads=[pst], writes=[Ut])
                    cur = Ut
                    st = 1
                    bufs = [SA, SB_]
                    bi = 0
                    while st < w:
                        nxt = bufs[bi]; bi ^= 1
                        k.op(DVE, lambda: nc.vector.tensor_tensor(out=nxt.ap[:, st:UW], in0=cur.ap[:, st:UW], in1=cur.ap[:, 0:UW - st], op=ALU.add), reads=[cur], writes=[nxt])
                        cur = nxt
                        st *= 2
                    pc = 2 * (g % 2) + cc
                    k.op(DVE, lambda: nc.vector.scalar_tensor_tensor(out=pT.ap[:, pc, :], in0=cur.ap[:, 16:UW], scalar=1.0 / w, in1=Ut.ap[:, 16:UW], op0=ALU.mult, op1=ALU.subtract), reads=[cur, Ut], writes=[pT])
                    k.op(DVE, lambda: nc.vector.tensor_tensor(out=tmp16.ap[:], in0=cur.ap[:, 16:32], in1=invc.ap[:, g, :], op=ALU.mult), reads=[cur, invc], writes=[tmp16])
                    k.op(DVE, lambda: nc.vector.tensor_tensor(out=pT.ap[:, pc, 0:16], in0=tmp16.ap[:], in1=Ut.ap[:, 16:32], op=ALU.subtract), reads=[tmp16, Ut], writes=[pT])
                for dc in range(2):
                    for n_ in range(2):
                        pst = ps[3 + n_]
                        k.mmg(pst, pst.ap[:], [(wpool.ap[:, g, cc, dc * 128:(dc + 1) * 128], pT.ap[:, 2 * (g % 2) + cc, n_ * 512:(n_ + 1) * 512]) for cc in range(2)], reads=[wpool, pT])
                        k.op(ACT, lambda: nc.scalar.activation(out=concatT.ap[:, 8 + 2 * g + dc, n_ * 512:(n_ + 1) * 512], in_=pst.ap[:], func=AF.Identity, scale=pscale.ap[:, 2 * g + dc:2 * g + dc + 1]), reads=[pst, pscale], writes=[concatT])
            k.barrier()

        x1 = [Tl(big[:, i * 2048:(i + 1) * 2048], f"x1_{i}") for i in range(8)]
        h2b = Tl(big[:, 16384:24576].bitcast(BF16).rearrange("p (i d) -> p i d", i=8), "h2b")
        w_o_v = w_o.rearrange("(c p) f -> p c f", p=128)
        with contextlib.ExitStack() as cs:
            g1b = Tl(cs.enter_context(nc.sbuf_tensor("g1b", [128, D], F32)), "g1b")
            tmpE = [Tl(cs.enter_context(nc.sbuf_tensor(f"tmpE{i}", [128, 512], F32)), f"tmpE{i}") for i in range(2)]
            k.dma(SP, g1b.ap[:], bcast_rows(mod_d.tensor, 2 * D, D), reads=[mod_dT], writes=[g1b])
            for i in range(8):
                k.dma(SP, x1[i].ap, x_own[i * 128:(i + 1) * 128, :], writes=[x1[i]])
            cnt = 0
            for nb in range(4):
                wb = wbuf[(nb + 2) % 3]
                k.dma(POOL, wb.ap[:], w_o_v[:, :, nb * 512:(nb + 1) * 512], writes=[wb])
                for i in range(8):
                    pst = ps[cnt % 2]; tm = tmpE[cnt % 2]; cnt += 1
                    k.mmg(pst, pst.ap[:], [(concatT.ap[:, kc, i * 128:(i + 1) * 128], wb.ap[:, kc, :]) for kc in range(16)], reads=[concatT, wb])
                    k.op(DVE, lambda: nc.vector.tensor_tensor(out=tm.ap[:], in0=pst.ap[:], in1=g1b.ap[:, nb * 512:(nb + 1) * 512], op=ALU.mult), reads=[pst, g1b], writes=[tm])
                    k.op(DVE, lambda: nc.vector.tensor_tensor(out=x1[i].ap[:, nb * 512:(nb + 1) * 512], in0=x1[i].ap[:, nb * 512:(nb + 1) * 512], in1=tm.ap[:], op=ALU.add), reads=[tm, x1[i]], writes=[x1[i]])
            k.barrier()
        concat_cm.__exit__(None, None, None)

        selb = Tl(sb("selb", [128, 8, NE], BF16), "selb")
        Pm = Tl(sb("Pm", [128, 8, NE], F32), "Pm")
        Gall = Tl(sb("Gall", [128, 8, NE], F32), "Gall")
        GHL = Tl(sb("GHL", [128, 8, NE, 2], BF16), "GHL")
        with contextlib.ExitStack() as cs:
            def sbc(name, shape, dtype):
                return Tl(cs.enter_context(nc.sbuf_tensor(name, shape, dtype)), name)
            A2b = sbc("A2b", [128, D], F32); B2b = sbc("B2b", [128, D], F32)
            n2b = sbc("n2b", [128, D], F32)
            h2fs = [sbc("h2f", [128, D], F32), n2b]
            h2Ts = [sbc(f"h2T{i}", [128, 16, 128], F32) for i in range(2)]
            wr = sbc("wr", [128, 16, NE], F32)
            brb = sbc("brb", [128, NE], F32)
            ssF = [sbc(f"ssF{i}", [128, 4], F32) for i in range(2)]
            lg = sbc("lg", [128, NE], F32); m8 = sbc("m8", [128, 8], F32); nm = sbc("nm", [128, 2], F32)
            selF = sbc("selF", [128, NE], F32); ex = sbc("ex", [128, NE], F32); gsum = sbc("gsum", [128, 2], F32)
            ghi = sbc("ghi", [128, NE], F32)
            psr = [ps[4], ps[6]]
            k.dma(SP, A2b.ap[:], bcast_rows(mod_d.tensor, 4 * D, D), reads=[mod_dT], writes=[A2b])
            k.dma(SP, B2b.ap[:], bcast_rows(mod_d.tensor, 3 * D, D), reads=[mod_dT], writes=[B2b])
            k.dma(SP, n2b.ap[:], bcast_rows(norm2_w.tensor, 0, D), writes=[n2b])
            k.dma(SP, wr.ap[:], w_router.rearrange("(c p) e -> p c e", p=128), writes=[wr])
            k.dma(SP, brb.ap[:], bcast_rows(b_router.tensor, 0, NE), writes=[brb])
            k.op(DVE, lambda: nc.vector.scalar_tensor_tensor(out=A2b.ap[:], in0=A2b.ap[:], scalar=1.0, in1=n2b.ap[:], op0=ALU.add, op1=ALU.mult), reads=[A2b, n2b], writes=[A2b])

            def front(i):
                ss = ssF[i % 2]; h2f = h2fs[i % 2]; h2T = h2Ts[i % 2]
                rms_rstd(x1[i], x1[i].ap, h2b.ap[:, i, :], ss, 0, D)
                k.dma(SP, x1_d[i * 128:(i + 1) * 128, :], x1[i].ap, reads=[x1[i]], writes=[x1_dT])
                k.op(DVE, lambda: nc.vector.scalar_tensor_tensor(out=h2f.ap[:], in0=x1[i].ap, scalar=ss.ap[:, 2:3], in1=A2b.ap[:], op0=ALU.mult, op1=ALU.mult), reads=[x1[i], ss, A2b], writes=[h2f])
                k.op(DVE, lambda: nc.vector.tensor_tensor(out=h2f.ap[:], in0=h2f.ap[:], in1=B2b.ap[:], op=ALU.add), reads=[h2f, B2b], writes=[h2f])
                k.op(ACT, lambda: nc.scalar.copy(out=h2b.ap[:, i, :], in_=h2f.ap[:]), reads=[h2f], writes=[h2b])
                for g in range(4):
                    pst = ps[g]
                    k.deps(PE, [h2f, ident_f], [pst])
                    ins = None
                    for j in range(4):
                        c = 4 * g + j
                        ins = nc.tensor.transpose(out=pst.ap[:, j * 128:(j + 1) * 128], in_=h2f.ap[:, c * 128:(c + 1) * 128], identity=ident_f.ap[:])
                    PE.n += 1; ins.then_inc(PE.sem, 1); tok = (PE.sem, PE.n, PE); k.finish(tok, [h2f, ident_f], [pst])
                    if g % 2 == 0:
                        k.op(ACT, lambda: nc.scalar.copy(out=h2T.ap[:, 4 * g:4 * g + 4, :], in_=pst.ap[:].rearrange("p (c t) -> p c t", c=4)), reads=[pst], writes=[h2T])
                    else:
                        k.op(DVE, lambda: nc.vector.tensor_copy(out=h2T.ap[:, 4 * g:4 * g + 4, :], in_=pst.ap[:].rearrange("p (c t) -> p c t", c=4)), reads=[pst], writes=[h2T])
                pr = psr[i % 2]
                k.mmg(pr, pr.ap[:, 0:NE], [(h2T.ap[:, kc, :], wr.ap[:, kc, :]) for kc in range(16)], reads=[h2T, wr])

            def back(i):
                pr = psr[i % 2]
                k.op(DVE, lambda: nc.vector.tensor_tensor(out=lg.ap[:], in0=pr.ap[:, 0:NE], in1=brb.ap[:], op=ALU.add), reads=[pr, brb], writes=[lg])
                k.op(DVE, lambda: nc.vector.max(out=m8.ap[:], in_=lg.ap[:]), reads=[lg], writes=[m8])
                k.op(DVE, lambda: nc.vector.tensor_scalar(out=selF.ap[:], in0=lg.ap[:], scalar1=m8.ap[:, 3:4], scalar2=None, op0=ALU.is_ge), reads=[lg, m8], writes=[selF])
                k.op(DVE, lambda: nc.vector.tensor_scalar(out=nm.ap[:, 0:1], in0=m8.ap[:, 0:1], scalar1=-1.0, scalar2=None, op0=ALU.mult), reads=[m8], writes=[nm])
                k.op(ACT, lambda: nc.scalar.activation(out=ex.ap[:], in_=lg.ap[:], func=AF.Exp, bias=nm.ap[:, 0:1], scale=1.0), reads=[lg, nm], writes=[ex])
                k.op(DVE, lambda: nc.vector.tensor_tensor(out=ex.ap[:], in0=ex.ap[:], in1=selF.ap[:], op=ALU.mult), reads=[ex, selF], writes=[ex])
                k.op(DVE, lambda: nc.vector.reduce_sum(out=gsum.ap[:, 0:1], in_=ex.ap[:], axis=mybir.AxisListType.X), reads=[ex], writes=[gsum])
                k.op(DVE, lambda: nc.vector.reciprocal(out=gsum.ap[:, 1:2], in_=gsum.ap[:, 0:1]), reads=[gsum], writes=[gsum])
                k.op(DVE, lambda: nc.vector.tensor_scalar(out=Gall.ap[:, i, :], in0=ex.ap[:], scalar1=gsum.ap[:, 1:2], scalar2=None, op0=ALU.mult), reads=[ex, gsum], writes=[Gall])
                k.op(DVE, lambda: nc.vector.tensor_copy(out=selb.ap[:, i, :], in_=selF.ap[:]), reads=[selF], writes=[selb])
                k.op(DVE, lambda: nc.vector.tensor_copy(out=GHL.ap[:, i, :, 0], in_=Gall.ap[:, i, :]), reads=[Gall], writes=[GHL])
                k.op(DVE, lambda: nc.vector.tensor_copy(out=ghi.ap[:], in_=GHL.ap[:, i, :, 0]), reads=[GHL], writes=[ghi])
                k.op(DVE, lambda: nc.vector.tensor_tensor(out=ghi.ap[:], in0=Gall.ap[:, i, :], in1=ghi.ap[:], op=ALU.subtract), reads=[Gall, ghi], writes=[ghi])
                k.op(DVE, lambda: nc.vector.tensor_copy(out=GHL.ap[:, i, :, 1], in_=ghi.ap[:]), reads=[ghi], writes=[GHL])
                ops_ = [(ones_b.ap[:], selb.ap[:, i2, :]) for i2 in range(i)] + [(strict_b.ap[:], selb.ap[:, i, :])]
                k.mmg(ps[5], ps[5].ap[:, 0:NE], ops_, reads=[ones_b, strict_b, selb])
                k.op(DVE, lambda: nc.vector.scalar_tensor_tensor(out=Pm.ap[:, i, :], in0=ps[5].ap[:, 0:NE], scalar=1.0, in1=selF.ap[:], op0=ALU.add, op1=ALU.mult), reads=[ps[5], selF], writes=[Pm])
                k.op(DVE, lambda: nc.vector.tensor_scalar(out=Pm.ap[:, i, :], in0=Pm.ap[:, i, :], scalar1=-1.0, scalar2=None, op0=ALU.add), reads=[Pm], writes=[Pm])

            front(0)
            for i in range(8):
                if i + 1 < 8:
                    front(i + 1)
                back(i)
            k.barrier()

        yacc = x1
        for i in range(8):
            k.op(DVE, lambda: nc.vector.memset(yacc[i].ap, 0.0), writes=[yacc[i]])

        with contextlib.ExitStack() as cs:
            def sbc(name, shape, dtype):
                return Tl(cs.enter_context(nc.sbuf_tensor(name, shape, dtype)), name)
            binT = sbc("binT", [128, NE * 32], F32)
            Pe = [sbc("Pe0", [128, 8, CAP], BF16)] * 2
            PT = sbc("PT", [128, 3, NT], BF16)
            XT = [sbc("XT0", [128, 16, CAP], BF16)] * 2
            HT = sbc("HT", [128, 16, CAP], BF16)
            Ygs = [sbc(f"Yg{i}", [128, 3, 512], BF16) for i in range(2)]
            gs = sbc("gs", [128, 4], F32)
            gcb = [sbc(f"gc{i}", [128, CAP], F32) for i in range(2)]
            sgb = [sbc("sg0", [128, CAP], F32)] * 2
            lcb = [sbc("lc0", [128, CAP], F32)] * 2
            stage = gcb[0]
            for r in range(8):
                k.dma(SP, stage.ap[:, 0:128], b_exp_in[r * 128:(r + 1) * 128, :], writes=[stage])
                k.deps(PE, [stage, ident_f], [ps[0]])
                ins = nc.tensor.transpose(out=ps[0].ap[:, 0:128], in_=stage.ap[:, 0:128], identity=ident_f.ap[:])
                PE.n += 1; ins.then_inc(PE.sem, 1); tok = (PE.sem, PE.n, PE); k.finish(tok, [stage, ident_f], [ps[0]])
                k.op(DVE, lambda: nc.vector.tensor_copy(out=binT.ap[:, r * 128:(r + 1) * 128], in_=ps[0].ap[:, 0:128]), reads=[ps[0]], writes=[binT])
            psb = Tl(ps[7].ap[:].bitcast(BF16), "psbf")
            SC = [(0, 128), (128, 128), (256, CAP - 256)]
            wq = []
            wcount = [0]

            def load_block(src_ap):
                wb = wbuf[wcount[0] % 3]; wcount[0] += 1
                k.dma(POOL, wb.ap[:], src_ap, writes=[wb])
                return wb

            w_in_e = w_exp_in.rearrange("e (c p) f -> e p c f", p=128)
            w_out_e = w_exp_out.rearrange("e (c p) f -> e p c f", p=128)

            def block_list(e):
                bl = []
                for j in range(4):
                    bl.append(("g", j, w_in_e[e, :, :, j * 512:(j + 1) * 512]))
                    bl.append(("l", j, w_in_e[e, :, :, D + j * 512:D + (j + 1) * 512]))
                for j in range(4):
                    bl.append(("o", j, w_out_e[e, :, :, j * 512:(j + 1) * 512]))
                return bl

            allblocks = []
            for e in range(N_EXPERTS_RUN):
                allblocks += [(e,) + b for b in block_list(e)]
            PREF = 2
            loaded = []
            for bi in range(min(PREF, len(allblocks))):
                loaded.append(load_block(allblocks[bi][3]))
            nloaded = [len(loaded)]

            def next_block(bi):
                nb_ = bi + PREF
                if nb_ < len(allblocks) and nloaded[0] <= nb_:
                    loaded.append(load_block(allblocks[nb_][3])); nloaded[0] += 1
                return loaded[bi]

            bi = 0
            upc = 0
            for e in range(N_EXPERTS_RUN):
                pe_t = Pe[e % 2]; xt_t = XT[e % 2]
                for i in range(8):
                    k.op(DVE, lambda: nc.vector.tensor_scalar(out=pe_t.ap[:, i, :], in0=iota_c.ap[:], scalar1=Pm.ap[:, i, e:e + 1], scalar2=None, op0=ALU.is_equal), reads=[iota_c, Pm], writes=[pe_t])
                for sc, (s0, sn) in enumerate(SC):
                    k.deps(PE, [pe_t, ident_b], [ps[7]])
                    ins = None
                    for i in range(8):
                        ins = nc.tensor.transpose(out=psb.ap[0:sn, i * 128:(i + 1) * 128], in_=pe_t.ap[:, i, s0:s0 + sn], identity=ident_b.ap[:])
                    PE.n += 1; ins.then_inc(PE.sem, 1); tok = (PE.sem, PE.n, PE); k.finish(tok, [pe_t, ident_b], [ps[7]])
                    k.op(ACT, lambda: nc.scalar.copy(out=PT.ap[0:sn, sc, :], in_=psb.ap[0:sn, :]), reads=[ps[7]], writes=[PT])
                    k.mmg(ps[6], ps[6].ap[0:sn, 0:2], [(pe_t.ap[:, i, s0:s0 + sn], GHL.ap[:, i, e, :]) for i in range(8)], reads=[pe_t, GHL])
                    k.op(DVE, lambda: nc.vector.reduce_sum(out=gs.ap[0:sn, sc:sc + 1], in_=ps[6].ap[0:sn, 0:2], axis=mybir.AxisListType.X), reads=[ps[6]], writes=[gs])
                for kc in range(16):
                    pst = ps[kc % 2]
                    k.mmg(pst, pst.ap[:, 0:CAP], [(h2b.ap[:, i, kc * 128:(kc + 1) * 128], pe_t.ap[:, i, :]) for i in range(8)], reads=[h2b, pe_t])
                    if kc % 2 == 0:
                        k.op(ACT, lambda: nc.scalar.copy(out=xt_t.ap[:, kc, :], in_=pst.ap[:, 0:CAP]), reads=[pst], writes=[xt_t])
                    else:
                        k.op(DVE, lambda: nc.vector.tensor_copy(out=xt_t.ap[:, kc, :], in_=pst.ap[:, 0:CAP]), reads=[pst], writes=[xt_t])
                for j in range(4):
                    wg = next_block(bi); bi += 1
                    for c in range(4):
                        fc = 4 * j + c
                        pst = ps[2 + upc % 2]; gc = gcb[upc % 2]; sg = sgb[upc % 2]; upc += 1
                        k.mmg(pst, pst.ap[:, 0:CAP], [(wg.ap[:, kc, c * 128:(c + 1) * 128], xt_t.ap[:, kc, :]) for kc in range(16)], reads=[wg, xt_t])
                        bcol = e * 32 + fc
                        k.op(DVE, lambda: nc.vector.tensor_scalar(out=gc.ap[:], in0=pst.ap[:, 0:CAP], scalar1=binT.ap[:, bcol:bcol + 1], scalar2=7.0, op0=ALU.add, op1=ALU.min), reads=[pst, binT], writes=[gc])
                        k.op(ACT, lambda: nc.scalar.activation(out=sg.ap[:], in_=gc.ap[:], func=AF.Sigmoid, scale=1.702), reads=[gc], writes=[sg])
                        k.op(DVE, lambda: nc.vector.tensor_tensor(out=HT.ap[:, fc, :], in0=gc.ap[:], in1=sg.ap[:], op=ALU.mult), reads=[gc, sg], writes=[HT])
                    wl = next_block(bi); bi += 1
                    for c in range(4):
                        fc = 4 * j + c
                        pst = ps[2 + upc % 2]; lc = lcb[upc % 2]; upc += 1
                        k.mmg(pst, pst.ap[:, 0:CAP], [(wl.ap[:, kc, c * 128:(c + 1) * 128], xt_t.ap[:, kc, :]) for kc in range(16)], reads=[wl, xt_t])
                        bcol = e * 32 + 16 + fc
                        k.op(DVE, lambda: nc.vector.tensor_scalar(out=lc.ap[:], in0=pst.ap[:, 0:CAP], scalar1=binT.ap[:, bcol:bcol + 1], scalar2=-7.0, op0=ALU.add, op1=ALU.max), reads=[pst, binT], writes=[lc])
                        k.op(DVE, lambda: nc.vector.tensor_scalar(out=lc.ap[:], in0=lc.ap[:], scalar1=7.0, scalar2=1.0, op0=ALU.min, op1=ALU.add), reads=[lc], writes=[lc])
                        k.op(DVE, lambda: nc.vector.tensor_tensor(out=HT.ap[:, fc, :], in0=HT.ap[:, fc, :], in1=lc.ap[:], op=ALU.mult), reads=[HT, lc], writes=[HT])
                def down(j):
                    wo_ = next_block(bi_box[0]); bi_box[0] += 1
                    Yg = Ygs[j % 2]
                    for sc, (s0, sn) in enumerate(SC):
                        pst = ps[4 + (3 * j + sc) % 2]
                        k.mmg(pst, pst.ap[0:sn, :], [(HT.ap[:, fcc, s0:s0 + sn], wo_.ap[:, fcc, :]) for fcc in range(16)], reads=[HT, wo_])
                        k.op(ACT, lambda: nc.scalar.activation(out=Yg.ap[0:sn, sc, :], in_=pst.ap[0:sn, :], func=AF.Identity, scale=gs.ap[0:sn, sc:sc + 1]), reads=[pst, gs], writes=[Yg])

                def scatter(j):
                    Yg = Ygs[j % 2]
                    for i in range(8):
                        pst = ps[i % 2]
                        k.mmg(pst, pst.ap[:], [(PT.ap[0:sn, sc, i * 128:(i + 1) * 128], Yg.ap[0:sn, sc, :]) for sc, (s0, sn) in enumerate(SC)], reads=[PT, Yg])
                        k.op(DVE, lambda: nc.vector.tensor_tensor(out=yacc[i].ap[:, j * 512:(j + 1) * 512], in0=yacc[i].ap[:, j * 512:(j + 1) * 512], in1=pst.ap[:], op=ALU.add), reads=[pst, yacc[i]], writes=[yacc[i]])
                bi_box = [bi]
                down(0)
                for j in range(4):
                    if j + 1 < 4:
                        down(j + 1)
                    scatter(j)
                bi = bi_box[0]
            k.barrier()

        with contextlib.ExitStack() as cs:
            def sbc(name, shape, dtype):
                return Tl(cs.enter_context(nc.sbuf_tensor(name, shape, dtype)), name)
            g2b = sbc("g2b", [128, D], F32)
            bo = sbc("bo", [NE, D], F32)
            xr = [sbc(f"xr{i}", [128, D], F32) for i in range(2)]
            k.dma(SP, g2b.ap[:], bcast_rows(mod_d.tensor, 5 * D, D), reads=[mod_dT], writes=[g2b])
            k.dma(SP, bo.ap[:], b_exp_out[:, :], writes=[bo])
            GT = sbc("GT", [NE, NT], F32)
            for i in range(8):
                k.deps(PE, [Gall, ident_f], [ps[6]])
                ins = nc.tensor.transpose(out=ps[6].ap[0:NE, 0:128], in_=Gall.ap[:, i, :], identity=ident_f.ap[:])
                PE.n += 1; ins.then_inc(PE.sem, 1); tok = (PE.sem, PE.n, PE); k.finish(tok, [Gall, ident_f], [ps[6]])
                k.op(ACT, lambda: nc.scalar.copy(out=GT.ap[:, i * 128:(i + 1) * 128], in_=ps[6].ap[0:NE, 0:128]), reads=[ps[6]], writes=[GT])
            cc_ = 0
            for i in range(8):
                xt = xr[i % 2]
                k.dma(SP, xt.ap[:], x1_d[i * 128:(i + 1) * 128, :], reads=[x1_dT], writes=[xt])
                for nb in range(4):
                    pst = ps[cc_ % 2]; cc_ += 1
                    k.mmg(pst, pst.ap[:], [(GT.ap[:, i * 128:(i + 1) * 128], bo.ap[:, nb * 512:(nb + 1) * 512])], reads=[GT, bo])
                    sl = slice(nb * 512, (nb + 1) * 512)
                    k.op(DVE, lambda: nc.vector.tensor_tensor(out=yacc[i].ap[:, sl], in0=yacc[i].ap[:, sl], in1=pst.ap[:], op=ALU.add), reads=[pst, yacc[i]], writes=[yacc[i]])
                k.op(DVE, lambda: nc.vector.tensor_tensor(out=yacc[i].ap, in0=yacc[i].ap, in1=g2b.ap[:], op=ALU.mult), reads=[yacc[i], g2b], writes=[yacc[i]])
                k.op(DVE, lambda: nc.vector.tensor_tensor(out=xt.ap[:], in0=xt.ap[:], in1=yacc[i].ap, op=ALU.add), reads=[yacc[i], xt], writes=[xt])
                k.dma(SP, out[i * 128:(i + 1) * 128, :], xt.ap[:], reads=[xt], writes=[outT])
            k.wait(SP, (outT.dsem, outT.dcnt, None))
            if DEBUG:
                k.wait(SP, (x1_dT.dsem, x1_dT.dcnt, None))

    return nc


def make_in_maps(inputs, ne_w=NE):
    f = lambda a: np.ascontiguousarray(np.asarray(a, dtype=np.float32))
    x = f(inputs["x"]); c = f(inputs["c"])
    shared = {
        "norm1_w": f(np.asarray(inputs["norm1_w"])[0].reshape(16, 128).T),
        "norm2_w": f(np.asarray(inputs["norm2_w"])[0]),
        "w_ada": f(np.asarray(inputs["w_ada"])[0]),
        "b_ada": f(np.asarray(inputs["b_ada"])[0]),
        "w_in": f(np.asarray(inputs["w_in"])[0]),
        "qk_w": f(np.stack([np.asarray(inputs["q_norm_w"])[0], np.asarray(inputs["k_norm_w"])[0]], axis=1)),
        "w_pool": f(np.asarray(inputs["w_pool"])[0]),
        "pool_scale": f(np.asarray(inputs["pool_scale"])[0].reshape(8, 128).T),
        "w_o": f(np.asarray(inputs["w_o"])[0]),
        "w_router": f(np.asarray(inputs["w_router"])[0]),
        "b_router": f(np.asarray(inputs["b_router"])[0]),
        "w_exp_in": f(np.asarray(inputs["w_exp_in"])[0][:ne_w]),
        "b_exp_in": f(np.asarray(inputs["b_exp_in"])[0].reshape(NE * 32, 128)),
        "w_exp_out": f(np.asarray(inputs["w_exp_out"])[0][:ne_w]),
        "b_exp_out": f(np.asarray(inputs["b_exp_out"])[0]),
    }
    in_maps = []
    for core in range(8):
        b, half = core // 2, core % 2
        fl = np.zeros((128, 2), np.float32)
        fl[:, 0] = 0.0 if half == 1 else NEGB
        fl[:, 1] = 1.0 if half == 1 else 0.0
        m = dict(shared)
        m["x_own"] = f(x[b, half * NT:(half + 1) * NT])
        m["x_prev"] = f(x[b, 0:NT])
        m["c_own"] = f(c[b].reshape(16, 128).T)
        m["flags"] = fl
        in_maps.append(m)
    return in_maps


_NC_CACHE = {}


def kernel(**inputs):
    in_maps = make_in_maps(inputs)
    if "nc" not in _NC_CACHE:
        _NC_CACHE["nc"] = build_nc()
    nc = _NC_CACHE["nc"]
    res = run_bass_kernel_spmd(nc, in_maps, core_ids=list(range(8)))
    outp = np.zeros((4, 2048, D), np.float32)
    for core in range(8):
        b, half = core // 2, core % 2
        outp[b, half * NT:(half + 1) * NT] = res.results[core]["out"]
    return outp
```

```python
import contextlib
import numpy as np
import concourse.bass as bass
import concourse.mybir as mybir
from concourse.bass_utils import run_bass_kernel_spmd

F32 = mybir.dt.float32
BF16 = mybir.dt.bfloat16
I32 = mybir.dt.int32
AF = mybir.ActivationFunctionType
ALU = mybir.AluOpType

D = 2048
NT = 1024
NE = 32
CAP = 352
EPS = 1e-6
NEGB = -30000.0


class Tl:
    __slots__ = ("ap", "w", "r", "dsem", "dcnt", "name")

    def __init__(s, ap, name=""):
        s.ap = ap; s.w = None; s.r = {}; s.dsem = None; s.dcnt = 0; s.name = name


class Eng:
    def __init__(s, k, name, eng, is_pe=False):
        s.eng = eng; s.sem = k.newsem(name); s.n = 0; s.waited = {}; s.is_pe = is_pe; s.name = name


class KB:
    def __init__(s, nc, es):
        s.nc = nc; s.es = es; s.nsem = 0; s.dsems = []
        s.pe = Eng(s, "pe", nc.tensor, True)
        s.act = Eng(s, "act", nc.scalar)
        s.dve = Eng(s, "dve", nc.vector)
        s.pool = Eng(s, "pool", nc.gpsimd)
        s.sp = Eng(s, "sp", nc.sync)
        s.engs = [s.pe, s.act, s.dve, s.pool, s.sp]

    def newsem(s, name):
        s.nsem += 1
        return s.es.enter_context(s.nc.semaphore(f"{name}_{s.nsem}"))

    def wait(s, e, tok):
        sem, val, src = tok
        if src is e:
            if e.is_pe or e.n - val >= 3:
                return
        key = id(sem)
        if e.waited.get(key, 0) >= val:
            return
        e.eng.wait_ge(sem, val)
        e.waited[key] = val

    def deps(s, e, reads, writes, dma_sem=None):
        for t in reads:
            if t.w is not None:
                s.wait(e, t.w)
        for t in writes:
            if t.w is not None and not (dma_sem is not None and t.w[0] is dma_sem):
                s.wait(e, t.w)
            for tok in t.r.values():
                s.wait(e, tok)

    def finish(s, tok, reads, writes):
        for t in writes:
            t.w = tok; t.r = {}
        for t in reads:
            key = id(tok[0]); old = t.r.get(key)
            if old is None or old[1] < tok[1]:
                t.r[key] = tok

    def op(s, e, fn, reads=(), writes=()):
        s.deps(e, reads, writes)
        ins = fn()
        e.n += 1
        ins.then_inc(e.sem, 1)
        tok = (e.sem, e.n, e)
        s.finish(tok, reads, writes)
        return tok

    def dma(s, e, out, in_, reads=(), writes=()):
        tgt = writes[0]
        if tgt.dsem is None:
            tgt.dsem = s.newsem("d" + tgt.name); s.dsems.append(tgt)
        s.deps(e, reads, writes, dma_sem=tgt.dsem)
        ins = e.eng.dma_start(out=out, in_=in_)
        tgt.dcnt += 16
        ins.then_inc(tgt.dsem, 16)
        tok = (tgt.dsem, tgt.dcnt, None)
        s.finish(tok, reads, writes)
        return tok

    def mmg(s, out_t, out_ap, ops, reads):
        nc = s.nc
        s.deps(s.pe, reads, [out_t])
        n = len(ops)
        ins = None
        for i, (l, r) in enumerate(ops):
            ins = nc.tensor.matmul(out=out_ap, lhsT=l, rhs=r, start=(i == 0), stop=(i == n - 1))
        s.pe.n += 1
        ins.then_inc(s.pe.sem, 1)
        tok = (s.pe.sem, s.pe.n, s.pe)
        s.finish(tok, reads, [out_t])
        return tok

    def barrier(s):
        for e in s.engs:
            for f in s.engs:
                if f is not e and f.n > 0:
                    s.wait(e, (f.sem, f.n, f))
            for t in s.dsems:
                if t.dcnt:
                    s.wait(e, (t.dsem, t.dcnt, None))


def bcast_rows(ap1d_tensor, offset, n, parts=128):
    return bass.AP(ap1d_tensor, offset, [[0, parts], [1, n]])


def build_nc(ne_w=NE, DEBUG=False):
    N_EXPERTS_RUN = ne_w
    nc = bass.Bass("TRN2", target_bir_lowering=False)
    dt = nc.dram_tensor
    x_own = dt("x_own", [NT, D], F32, kind="ExternalInput").ap()
    x_prev = dt("x_prev", [NT, D], F32, kind="ExternalInput").ap()
    c_own = dt("c_own", [128, 16], F32, kind="ExternalInput").ap()
    flags = dt("flags", [128, 2], F32, kind="ExternalInput").ap()
    norm1_w = dt("norm1_w", [128, 16], F32, kind="ExternalInput").ap()
    norm2_w = dt("norm2_w", [D], F32, kind="ExternalInput").ap()
    w_ada = dt("w_ada", [D, 6 * D], F32, kind="ExternalInput").ap()
    b_ada = dt("b_ada", [6 * D], F32, kind="ExternalInput").ap()
    w_in = dt("w_in", [D, 2 * D], F32, kind="ExternalInput").ap()
    qk_w = dt("qk_w", [128, 2], F32, kind="ExternalInput").ap()
    w_pool = dt("w_pool", [4, 256, 256], F32, kind="ExternalInput").ap()
    pool_scale = dt("pool_scale", [128, 8], F32, kind="ExternalInput").ap()
    w_o = dt("w_o", [D, D], F32, kind="ExternalInput").ap()
    w_router = dt("w_router", [D, NE], F32, kind="ExternalInput").ap()
    b_router = dt("b_router", [NE], F32, kind="ExternalInput").ap()
    w_exp_in = dt("w_exp_in", [ne_w, D, 2 * D], F32, kind="ExternalInput").ap()
    b_exp_in = dt("b_exp_in", [NE * 32, 128], F32, kind="ExternalInput").ap()
    w_exp_out = dt("w_exp_out", [ne_w, D, D], F32, kind="ExternalInput").ap()
    b_exp_out = dt("b_exp_out", [NE, D], F32, kind="ExternalInput").ap()
    out = dt("out", [NT, D], F32, kind="ExternalOutput").ap()
    mod_d = dt("mod_d", [6 * D], F32, kind="Internal").ap()
    x1_d = dt("x1_d", [NT, D], F32, kind="ExternalOutput" if DEBUG else "Internal").ap()

    with contextlib.ExitStack() as es:
        k = KB(nc, es)
        PE, ACT, DVE, POOL, SP = k.pe, k.act, k.dve, k.pool, k.sp

        def sb(name, shape, dtype):
            return es.enter_context(nc.sbuf_tensor(name, shape, dtype))

        wbuf = [Tl(sb(f"wbuf{i}", [128, 16, 512], BF16), f"wbuf{i}") for i in range(3)]
        big = sb("big", [128, 24576], F32)
        ident_f = Tl(sb("ident_f", [128, 128], F32), "identf")
        ident_b = Tl(sb("ident_b", [128, 128], BF16), "identb")
        tri_b = Tl(sb("tri_b", [128, 128], BF16), "tri")
        ones_b = Tl(sb("ones_b", [128, 128], BF16), "ones")
        strict_b = Tl(sb("strict_b", [128, 128], BF16), "strict")
        iota_c = Tl(sb("iota_c", [128, CAP], F32), "iotac")
        small = Tl(sb("small", [128, 64], F32), "small")
        modT = Tl(sb("modT", [128, 96], F32), "modT")
        flg = Tl(sb("flg", [128, 2], F32), "flg")
        ps = [Tl(es.enter_context(nc.psum_tensor(f"ps{i}", [128, 512], F32)), f"ps{i}") for i in range(8)]
        mod_dT = Tl(mod_d, "mod_d")
        x1_dT = Tl(x1_d, "x1_d")
        outT = Tl(out, "out")

        ZC = small.ap[:, 0:1]

        with contextlib.ExitStack() as cs:
            ii = Tl(cs.enter_context(nc.sbuf_tensor("ii", [128, 512], I32)), "ii")
            ff = Tl(cs.enter_context(nc.sbuf_tensor("ff", [128, 512], F32)), "ff")
            k.op(POOL, lambda: nc.gpsimd.iota(ii.ap[:, 0:128], pattern=[[1, 128]], base=0, channel_multiplier=-1), writes=[ii])
            k.op(DVE, lambda: nc.vector.tensor_copy(out=ff.ap[:, 0:128], in_=ii.ap[:, 0:128]), reads=[ii], writes=[ff])
            k.op(DVE, lambda: nc.vector.tensor_scalar(out=ident_f.ap[:], in0=ff.ap[:, 0:128], scalar1=0.0, scalar2=None, op0=ALU.is_equal), reads=[ff], writes=[ident_f])
            k.op(DVE, lambda: nc.vector.tensor_scalar(out=ident_b.ap[:], in0=ff.ap[:, 0:128], scalar1=0.0, scalar2=None, op0=ALU.is_equal), reads=[ff], writes=[ident_b])
            k.op(DVE, lambda: nc.vector.tensor_scalar(out=tri_b.ap[:], in0=ff.ap[:, 0:128], scalar1=0.0, scalar2=None, op0=ALU.is_le), reads=[ff], writes=[tri_b])
            k.op(DVE, lambda: nc.vector.tensor_scalar(out=strict_b.ap[:], in0=ff.ap[:, 0:128], scalar1=0.0, scalar2=None, op0=ALU.is_gt), reads=[ff], writes=[strict_b])
            k.op(DVE, lambda: nc.vector.memset(ones_b.ap[:], 1.0), writes=[ones_b])
            k.op(DVE, lambda: nc.vector.memset(small.ap[:], 0.0), writes=[small])
            k.op(POOL, lambda: nc.gpsimd.iota(ii.ap[:, 0:CAP], pattern=[[1, CAP]], base=0, channel_multiplier=0), reads=[], writes=[ii])
            k.op(DVE, lambda: nc.vector.tensor_copy(out=iota_c.ap[:], in_=ii.ap[:, 0:CAP]), reads=[ii], writes=[iota_c])
            k.dma(SP, flg.ap[:], flags[:, :], writes=[flg])
            k.barrier()

        cTb = Tl(sb("cTb", [128, 16], BF16), "cTb")
        stg = Tl(sb("stg", [1, 512], F32), "stg")
        bst = Tl(sb("bst", [1, 512], F32), "bst")
        wav = w_ada.rearrange("(c p) f -> p c f", p=128)

        def mod_load(nb, wb):
            k.dma(POOL, wb.ap[:], wav[:, :, nb * 512:(nb + 1) * 512], writes=[wb])

        def mod_block(nb, wb, pst, load=True):
            k.dma(SP, bst.ap[:], bass.AP(b_ada.tensor, nb * 512, [[0, 1], [1, 512]]), writes=[bst])
            if load:
                mod_load(nb, wb)
            k.mmg(pst, pst.ap[0:1, :], [(cTb.ap[:, kc:kc + 1], wb.ap[:, kc, :]) for kc in range(16)], reads=[cTb, wb])
            k.op(DVE, lambda: nc.vector.tensor_tensor(out=stg.ap[:], in0=pst.ap[0:1, :], in1=bst.ap[:], op=ALU.add), reads=[pst, bst], writes=[stg])
            k.dma(SP, bass.AP(mod_d.tensor, nb * 512, [[0, 1], [1, 512]]), stg.ap[:], reads=[stg], writes=[mod_dT])

        with contextlib.ExitStack() as cs:
            cT = Tl(cs.enter_context(nc.sbuf_tensor("cT", [128, 16], F32)), "cT")
            k.dma(SP, cT.ap[:], c_own[:, :], writes=[cT])
            k.op(ACT, lambda: nc.scalar.activation(out=cTb.ap[:], in_=cT.ap[:], func=AF.Silu), reads=[cT], writes=[cTb])
            for nb in range(8):
                mod_block(nb, wbuf[nb % 3], ps[nb % 2])
            with nc.allow_non_contiguous_dma(reason="small one-time relayout of the modulation vector"):
                k.dma(SP, modT.ap[:, 0:32], bass.AP(mod_d.tensor, 0, [[1, 128], [128, 32]]), reads=[mod_dT], writes=[modT])
            k.barrier()

        A1T = Tl(sb("A1T", [128, 16], F32), "A1T")
        n1w = Tl(sb("n1w", [128, 16], F32), "n1w")
        k.dma(SP, n1w.ap[:], norm1_w[:, :], writes=[n1w])
        k.op(DVE, lambda: nc.vector.scalar_tensor_tensor(out=A1T.ap[:], in0=modT.ap[:, 16:32], scalar=1.0, in1=n1w.ap[:], op0=ALU.add, op1=ALU.mult), reads=[modT, n1w], writes=[A1T])
        qkw = Tl(sb("qkw", [128, 2], F32), "qkw")
        k.dma(SP, qkw.ap[:], qk_w[:, :], writes=[qkw])
        k.op(DVE, lambda: nc.vector.tensor_scalar(out=small.ap[:, 2:3], in0=qkw.ap[:, 0:1], scalar1=float(128 ** -0.5), scalar2=None, op0=ALU.mult), reads=[qkw], writes=[small])
        k.op(DVE, lambda: nc.vector.tensor_copy(out=small.ap[:, 3:4], in_=qkw.ap[:, 1:2]), reads=[qkw], writes=[small])

        hT_ap = big[:, 0:16384].bitcast(BF16).rearrange("p (c t) -> p c t", c=16)
        hT = Tl(hT_ap, "hT")
        spare = big[:, 16384:24576]

        def rms_rstd(src_t, src_ap, junk_ap, ss_t, col, n):
            k.op(DVE, lambda: nc.vector.memset(ss_t.ap[:, col:col + 1], 0.0), writes=[ss_t])
            k.op(ACT, lambda: nc.scalar.activation(out=junk_ap, in_=src_ap, func=AF.Square, accum_out=ss_t.ap[:, col:col + 1]), reads=[src_t, ss_t], writes=[ss_t])
            k.op(DVE, lambda: nc.vector.tensor_scalar(out=ss_t.ap[:, col + 1:col + 2], in0=ss_t.ap[:, col:col + 1], scalar1=1.0 / n, scalar2=EPS, op0=ALU.mult, op1=ALU.add), reads=[ss_t], writes=[ss_t])
            k.op(ACT, lambda: nc.scalar.activation(out=ss_t.ap[:, col + 1:col + 2], in_=ss_t.ap[:, col + 1:col + 2], func=AF.Sqrt), reads=[ss_t], writes=[ss_t])
            k.op(DVE, lambda: nc.vector.reciprocal(out=ss_t.ap[:, col + 2:col + 3], in_=ss_t.ap[:, col + 1:col + 2]), reads=[ss_t], writes=[ss_t])

        concat_cm = nc.sbuf_tensor("concatT", [128, 16, NT], BF16)
        concatT = Tl(concat_cm.__enter__(), "concatT")

        with contextlib.ExitStack() as cs:
            xts = [Tl(spare[:, i * 2048:(i + 1) * 2048], f"xt{i}") for i in range(2)]
            junk = Tl(spare[:, 4096:5120].bitcast(BF16), "junk")
            sst = [Tl(cs.enter_context(nc.sbuf_tensor(f"ssB{i}", [128, 4], F32)), f"ssB{i}") for i in range(2)]
            for ti in range(16):
                src = x_prev if ti < 8 else x_own
                r0 = (ti % 8) * 128
                xt = xts[ti % 2]; ss = sst[ti % 2]
                k.dma(SP, xt.ap, src[r0:r0 + 128, :], writes=[xt])
                rms_rstd(xt, xt.ap, junk.ap, ss, 0, D)
                k.op(DVE, lambda: nc.vector.tensor_scalar(out=xt.ap, in0=xt.ap, scalar1=ss.ap[:, 2:3], scalar2=None, op0=ALU.mult), reads=[xt, ss], writes=[xt])
                for g in range(4):
                    pst = ps[g]
                    k.deps(PE, [xt, ident_f], [pst])
                    ins = None
                    for j in range(4):
                        c = 4 * g + j
                        ins = nc.tensor.transpose(out=pst.ap[:, j * 128:(j + 1) * 128], in_=xt.ap[:, c * 128:(c + 1) * 128], identity=ident_f.ap[:])
                    PE.n += 1; ins.then_inc(PE.sem, 1); tok = (PE.sem, PE.n, PE); k.finish(tok, [xt, ident_f], [pst])
                    for j in range(4):
                        c = 4 * g + j
                        if j % 2 == 0:
                            k.op(ACT, lambda: nc.scalar.activation(out=hT.ap[:, c, ti * 128:(ti + 1) * 128], in_=pst.ap[:, j * 128:(j + 1) * 128], func=AF.Identity, scale=A1T.ap[:, c:c + 1], bias=modT.ap[:, c:c + 1]), reads=[pst, A1T, modT], writes=[hT])
                        else:
                            k.op(DVE, lambda: nc.vector.tensor_scalar(out=hT.ap[:, c, ti * 128:(ti + 1) * 128], in0=pst.ap[:, j * 128:(j + 1) * 128], scalar1=A1T.ap[:, c:c + 1], scalar2=modT.ap[:, c:c + 1], op0=ALU.mult, op1=ALU.add), reads=[pst, A1T, modT], writes=[hT])
            k.barrier()

        w_in_v = w_in.rearrange("(c p) f -> p c f", p=128)
        with contextlib.ExitStack() as cs:
            def sbc(name, shape, dtype):
                return Tl(cs.enter_context(nc.sbuf_tensor(name, shape, dtype)), name)
            NEG = Tl(spare[:, 0:2048].rearrange("p (j t) -> p j t", j=4), "NEG")
            QT = [Tl(spare[:, 2048 + i * 512:2048 + (i + 1) * 512].bitcast(BF16), f"QT{i}") for i in range(2)]
            KT = [Tl(spare[:, 3072 + i * 1024:3072 + (i + 1) * 1024].bitcast(BF16), f"KT{i}") for i in range(2)]
            VV = [Tl(spare[:, 5120 + i * 1024:5120 + (i + 1) * 1024].bitcast(BF16).rearrange("p (t d) -> p t d", t=16), f"V{i}") for i in range(2)]
            sq = [sbc("sq0", [128, 512], BF16)] * 2
            rstd = [sbc("rstd0", [128, 512], F32)] * 2
            vtmp = sbc("vtmp", [128, 512], BF16)
            Eb = [sbc(f"E{i}", [128, 512], F32) for i in range(2)]
            Lb = [sbc(f"L{i}", [128, 512], BF16) for i in range(2)]
            T1 = sbc("T1", [128, 512], F32)
            Zmb = [sbc(f"Zm{i}", [128, 512], F32) for i in range(2)]
            T2 = [sbc("T20", [128, 512], F32)] * 2
            Ab = [sbc(f"A{i}", [128, 512], BF16) for i in range(2)]
            Rb = sbc("Rb", [128, 512], F32)
            ps2b = ps[2].ap[:].bitcast(BF16)
            ii = Tl(T1.ap[:].bitcast(I32), "iiC")
            for j in range(4):
                k.op(POOL, lambda: nc.gpsimd.iota(ii.ap, pattern=[[1, 512]], base=-128 * j, channel_multiplier=-1), writes=[ii])
                k.op(DVE, lambda: nc.vector.tensor_copy(out=NEG.ap[:, j, :], in_=ii.ap), reads=[ii], writes=[NEG])
                k.op(DVE, lambda: nc.vector.tensor_scalar(out=NEG.ap[:, j, :], in0=NEG.ap[:, j, :], scalar1=0.0, scalar2=NEGB, op0=ALU.is_le, op1=ALU.mult), reads=[NEG], writes=[NEG])
            k.barrier()

            def qk_norm(pst, dst_t, dst_ap, wcol):
                sqt = sq[0]; rs = rstd[0]
                k.op(ACT, lambda: nc.scalar.activation(out=sqt.ap[:], in_=pst.ap[:], func=AF.Square), reads=[pst], writes=[sqt])
                k.mmg(ps[2], ps[2].ap[:], [(ones_b.ap[:], sqt.ap[:])], reads=[ones_b, sqt])
                k.op(ACT, lambda: nc.scalar.activation(out=rs.ap[:], in_=ps[2].ap[:], func=AF.Ln, scale=1.0 / 128, bias=small.ap[:, 5:6]), reads=[ps[2], small], writes=[rs])
                k.op(ACT, lambda: nc.scalar.activation(out=rs.ap[:], in_=rs.ap[:], func=AF.Exp, scale=-0.5), reads=[rs], writes=[rs])
                k.op(DVE, lambda: nc.vector.scalar_tensor_tensor(out=dst_ap, in0=pst.ap[:], scalar=small.ap[:, wcol:wcol + 1], in1=rs.ap[:], op0=ALU.mult, op1=ALU.mult), reads=[pst, rs, small], writes=[dst_t])

            k.op(DVE, lambda: nc.vector.memset(small.ap[:, 5:6], EPS), writes=[small])
            acc_ctr = [0]

            def inproj_items(h):
                wb = wbuf[h % 2]; qt = QT[h % 2]; kt = KT[h % 2]; vt = VV[h % 2]
                items = []

                def it_w():
                    for j, c0_ in enumerate((128 * h, 1024 + 128 * h, 2048 + 128 * h)):
                        k.dma(POOL, wb.ap[:, :, j * 128:(j + 1) * 128], w_in_v[:, :, c0_:c0_ + 128], writes=[wb])
                items.append(it_w)

                def mk_q(n_):
                    def f():
                        pst = ps[acc_ctr[0] % 2]; acc_ctr[0] += 1
                        k.mmg(pst, pst.ap[:], [(wb.ap[:, kc, 0:128], hT.ap[:, kc, 1024 + n_ * 512:1024 + (n_ + 1) * 512]) for kc in range(16)], reads=[wb, hT])
                        qk_norm(pst, qt, qt.ap[:, n_ * 512:(n_ + 1) * 512], 2)
                    return f

                def mk_k(n_):
                    def f():
                        pst = ps[acc_ctr[0] % 2]; acc_ctr[0] += 1
                        k.mmg(pst, pst.ap[:], [(wb.ap[:, kc, 128:256], hT.ap[:, kc, n_ * 512:(n_ + 1) * 512]) for kc in range(16)], reads=[wb, hT])
                        qk_norm(pst, kt, kt.ap[:, n_ * 512:(n_ + 1) * 512], 3)
                    return f

                def mk_v(n_):
                    def f():
                        pst = ps[acc_ctr[0] % 2]; acc_ctr[0] += 1
                        k.mmg(pst, pst.ap[:], [(wb.ap[:, kc, 256:384], hT.ap[:, kc, n_ * 512:(n_ + 1) * 512]) for kc in range(16)], reads=[wb, hT])
                        k.op(DVE, lambda: nc.vector.tensor_copy(out=vtmp.ap[:], in_=pst.ap[:]), reads=[pst], writes=[vtmp])
                        k.deps(PE, [vtmp, ident_b], [ps[2]])
                        ins = None
                        for j in range(4):
                            ins = nc.tensor.transpose(out=ps2b[:, j * 128:(j + 1) * 128], in_=vtmp.ap[:, j * 128:(j + 1) * 128], identity=ident_b.ap[:])
                        PE.n += 1; ins.then_inc(PE.sem, 1); tok = (PE.sem, PE.n, PE); k.finish(tok, [vtmp, ident_b], [ps[2]])
                        k.op(DVE, lambda: nc.vector.tensor_copy(out=vt.ap[:, 4 * n_:4 * n_ + 4, :], in_=ps2b[:, 0:512].rearrange("p (t d) -> p t d", t=4)), reads=[ps[2]], writes=[vt])
                    return f
                items += [mk_k(0), mk_q(0), mk_k(1), mk_v(0), mk_k(2), mk_v(1), mk_q(1), mk_k(3), mk_v(2), mk_v(3)]
                return items

            mod_next = [8]

            mod_load(8, wbuf[2])

            def mod_item():
                if mod_next[0] < 24:
                    mod_block(mod_next[0], wbuf[2], ps[2], load=False); mod_next[0] += 1
                    if mod_next[0] < 24:
                        mod_load(mod_next[0], wbuf[2])

            for f in inproj_items(0):
                f()
            for h in range(8):
                qt = QT[h % 2]; kt = KT[h % 2]; vt = VV[h % 2]
                pending = inproj_items(h + 1) if h < 7 else []
                gstep = 0
                for qc in range(2):
                    steps = []
                    for j in (3, 2, 1, 0):
                        kb = 4 * qc + j
                        steps.append((1024 + kb * 128, 8 + kb, j, False))
                    for kb in range(4 * qc - 1, -1, -1):
                        steps.append((1024 + kb * 128, 8 + kb, None, False))
                    for kb in range(7, -1, -1):
                        steps.append((kb * 128, kb, None, True))
                    n = len(steps)
                    k.op(DVE, lambda: nc.vector.memset(Rb.ap[:], 0.0), writes=[Rb])
                    qap = qt.ap[:, qc * 512:(qc + 1) * 512]
                    oT = ps[7]
                    zsrc = [None] * n
                    mod_item()
                    for it in range(n + 2):
                        gstep += 1
                        if pending and (gstep == 1 or (gstep >= 6 and gstep % 2 == 0)):
                            pending.pop(0)()
                        if it < n:
                            i = it
                            kcol, vti, dj, isprev = steps[i]
                            Z = ps[3 + i % 2]; E = Eb[i % 2]; L = Lb[i % 2]
                            k.mmg(Z, Z.ap[:], [(kt.ap[:, kcol:kcol + 128], qap)], reads=[kt, qt])
                            bias_ap = flg.ap[:, 0:1] if isprev else ZC
                            if dj is not None:
                                Zm = Zmb[i % 2]
                                k.op(DVE, lambda: nc.vector.tensor_tensor(out=Zm.ap[:], in0=Z.ap[:], in1=NEG.ap[:, dj, :], op=ALU.add), reads=[Z, NEG], writes=[Zm])
                                zs = Zm
                            else:
                                zs = Z
                            zsrc[i] = zs
                            k.op(ACT, lambda: nc.scalar.activation(out=E.ap[:], in_=zs.ap[:], func=AF.Exp, bias=bias_ap, scale=1.0), reads=[zs, flg, small], writes=[E])
                            k.op(ACT, lambda: nc.scalar.activation(out=L.ap[:], in_=E.ap[:], func=AF.Ln, bias=1.0, scale=1.0), reads=[E], writes=[L])
                        if 0 <= it - 1 < n:
                            i = it - 1
                            kcol, vti, dj, isprev = steps[i]
                            E = Eb[i % 2]; L = Lb[i % 2]; A = Ab[i % 2]; T2t = T2[i % 2]
                            zs = zsrc[i]
                            bias_ap = flg.ap[:, 0:1] if isprev else ZC
                            k.mmg(ps[5], ps[5].ap[:], [(tri_b.ap[:], L.ap[:])], reads=[tri_b, L])
                            if i < n - 1:
                                k.mmg(ps[6], ps[6].ap[:], [(ones_b.ap[:], L.ap[:])], reads=[ones_b, L])
                            k.op(DVE, lambda: nc.vector.tensor_tensor(out=T1.ap[:], in0=zs.ap[:], in1=Rb.ap[:], op=ALU.subtract), reads=[zs, Rb], writes=[T1])
                            k.op(DVE, lambda: nc.vector.tensor_tensor(out=T2t.ap[:], in0=T1.ap[:], in1=ps[5].ap[:], op=ALU.subtract), reads=[T1, ps[5]], writes=[T2t])
                            if i < n - 1:
                                k.op(DVE, lambda: nc.vector.tensor_tensor(out=Rb.ap[:], in0=Rb.ap[:], in1=ps[6].ap[:], op=ALU.add), reads=[Rb, ps[6]], writes=[Rb])
                            k.op(ACT, lambda: nc.scalar.activation(out=A.ap[:], in_=T2t.ap[:], func=AF.Exp, bias=bias_ap, scale=1.0), reads=[T2t, flg, small], writes=[A])
                        if it - 2 >= 0:
                            i = it - 2
                            kcol, vti, dj, isprev = steps[i]
                            A = Ab[i % 2]
                            k.deps(PE, [vt, A], [oT])
                            ins = nc.tensor.matmul(out=oT.ap[:], lhsT=vt.ap[:, vti, :], rhs=A.ap[:], start=(i == 0), stop=(i == n - 1))
                            PE.n += 1; ins.then_inc(PE.sem, 1); tok = (PE.sem, PE.n, PE); k.finish(tok, [vt, A], [oT])
                    k.op(ACT, lambda: nc.scalar.copy(out=concatT.ap[:, h, qc * 512:(qc + 1) * 512], in_=oT.ap[:]), reads=[oT], writes=[concatT])
                while pending:
                    pending.pop(0)()
            while mod_next[0] < 24:
                mod_item()
            k.barrier()

        with contextlib.ExitStack() as cs:
            def sbc(name, shape, dtype):
                return Tl(cs.enter_context(nc.sbuf_tensor(name, shape, dtype)), name)
            UW = 16 + NT
            U = [Tl(spare[:, i * 1040:(i + 1) * 1040], f"U{i}") for i in range(2)]
            SA = Tl(spare[:, 2080:3120], "SA"); SB_ = Tl(spare[:, 3120:4160], "SB")
            pT = Tl(spare[:, 4160:6208].bitcast(BF16).rearrange("p (c t) -> p c t", c=4), "pT")
            wpool = sbc("wpool", [128, 4, 2, 256], BF16)
            pscale = sbc("pscale", [128, 8], F32)
            invc = sbc("invc", [128, 4, 16], F32)
            tmp16 = sbc("tmp16", [128, 16], F32)
            iiD = sbc("iiD", [128, 16], I32)
            k.dma(POOL, wpool.ap[:], w_pool.rearrange("g (c p) d -> p g c d", p=128), writes=[wpool])
            k.dma(SP, pscale.ap[:], pool_scale[:, :], writes=[pscale])
            k.op(POOL, lambda: nc.gpsimd.iota(iiD.ap[:], pattern=[[1, 16]], base=1, channel_multiplier=0), writes=[iiD])
            k.op(DVE, lambda: nc.vector.tensor_copy(out=tmp16.ap[:], in_=iiD.ap[:]), reads=[iiD], writes=[tmp16])
            k.op(DVE, lambda: nc.vector.tensor_scalar(out=small.ap[:, 4:5], in0=flg.ap[:, 1:2], scalar1=1024.0, scalar2=None, op0=ALU.mult), reads=[flg], writes=[small])
            k.op(DVE, lambda: nc.vector.tensor_scalar(out=tmp16.ap[:], in0=tmp16.ap[:], scalar1=small.ap[:, 4:5], scalar2=None, op0=ALU.add), reads=[small, tmp16], writes=[tmp16])
            for g in range(4):
                k.op(DVE, lambda: nc.vector.tensor_scalar(out=invc.ap[:, g, :], in0=tmp16.ap[:], scalar1=float(2 ** (g + 1)), scalar2=None, op0=ALU.min), reads=[tmp16], writes=[invc])
            k.op(DVE, lambda: nc.vector.reciprocal(out=invc.ap[:], in_=invc.ap[:]), reads=[invc], writes=[invc])
            for g in range(4):
                w = 2 ** (g + 1)
                wb = wbuf[g % 2]
                k.dma(POOL, wb.ap[:, :, 0:256], w_in_v[:, :, 3072 + 256 * g:3072 + 256 * (g + 1)], writes=[wb])
                for cc in range(2):
                    Ut = U[cc]
                    pst = ps[2]
                    k.mmg(pst, pst.ap[:, 0:128], [(wb.ap[:, kc, cc * 128:(cc + 1) * 128], hT.ap[:, kc, 896:1024]) for kc in range(16)], reads=[wb, hT])
                    k.op(DVE, lambda: nc.vector.tensor_scalar(out=Ut.ap[:, 0:16], in0=pst.ap[:, 112:128], scalar1=flg.ap[:, 1:2], scalar2=None, op0=ALU.mult), reads=[pst, flg], writes=[Ut])
                    for n_ in range(2):
                        pst = ps[n_ % 2]
                        k.mmg(pst, pst.ap[:], [(wb.ap[:, kc, cc * 128:(cc + 1) * 128], hT.ap[:, kc, 1024 + n_ * 512:1024 + (n_ + 1) * 512]) for kc in range(16)], reads=[wb, hT])
                        k.op(ACT, lambda: nc.scalar.copy(out=Ut.ap[:, 16 + n_ * 512:16 + (n_ + 1) * 512], in_=pst.ap[:]), reads=[pst], writes=[Ut])
                    cur = Ut
                    st = 1
                    bufs = [SA, SB_]
                    bi = 0
                    while st < w:
                        nxt = bufs[bi]; bi ^= 1
                        k.op(DVE, lambda: nc.vector.tensor_tensor(out=nxt.ap[:, st:UW], in0=cur.ap[:, st:UW], in1=cur.ap[:, 0:UW - st], op=ALU.add), reads=[cur], writes=[nxt])
                        cur = nxt
                        st *= 2
                    pc = 2 * (g % 2) + cc
                    k.op(DVE, lambda: nc.vector.scalar_tensor_tensor(out=pT.ap[:, pc, :], in0=cur.ap[:, 16:UW], scalar=1.0 / w, in1=Ut.ap[:, 16:UW], op0=ALU.mult, op1=ALU.subtract), reads=[cur, Ut], writes=[pT])
                    k.op(DVE, lambda: nc.vector.tensor_tensor(out=tmp16.ap[:], in0=cur.ap[:, 16:32], in1=invc.ap[:, g, :], op=ALU.mult), reads=[cur, invc], writes=[tmp16])
                    k.op(DVE, lambda: nc.vector.tensor_tensor(out=pT.ap[:, pc, 0:16], in0=tmp16.ap[:], in1=Ut.ap[:, 16:32], op=ALU.subtract), reads=[tmp16, Ut], writes=[pT])
                for dc in range(2):
                    for n_ in range(2):
                        pst = ps[3 + n_]
                        k.mmg(pst, pst.ap[:], [(wpool.ap[:, g, cc, dc * 128:(dc + 1) * 128], pT.ap[:, 2 * (g % 2) + cc, n_ * 512:(n_ + 1) * 512]) for cc in range(2)], reads=[wpool, pT])
                        k.op(ACT, lambda: nc.scalar.activation(out=concatT.ap[:, 8 + 2 * g + dc, n_ * 512:(n_ + 1) * 512], in_=pst.ap[:], func=AF.Identity, scale=pscale.ap[:, 2 * g + dc:2 * g + dc + 1]), reads=[pst, pscale], writes=[concatT])
            k.barrier()

        x1 = [Tl(big[:, i * 2048:(i + 1) * 2048], f"x1_{i}") for i in range(8)]
        h2b = Tl(big[:, 16384:24576].bitcast(BF16).rearrange("p (i d) -> p i d", i=8), "h2b")
        w_o_v = w_o.rearrange("(c p) f -> p c f", p=128)
        with contextlib.ExitStack() as cs:
            g1b = Tl(cs.enter_context(nc.sbuf_tensor("g1b", [128, D], F32)), "g1b")
            tmpE = [Tl(cs.enter_context(nc.sbuf_tensor(f"tmpE{i}", [128, 512], F32)), f"tmpE{i}") for i in range(2)]
            k.dma(SP, g1b.ap[:], bcast_rows(mod_d.tensor, 2 * D, D), reads=[mod_dT], writes=[g1b])
            for i in range(8):
                k.dma(SP, x1[i].ap, x_own[i * 128:(i + 1) * 128, :], writes=[x1[i]])
            cnt = 0
            for nb in range(4):
                wb = wbuf[(nb + 2) % 3]
                k.dma(POOL, wb.ap[:], w_o_v[:, :, nb * 512:(nb + 1) * 512], writes=[wb])
                for i in range(8):
                    pst = ps[cnt % 2]; tm = tmpE[cnt % 2]; cnt += 1
                    k.mmg(pst, pst.ap[:], [(concatT.ap[:, kc, i * 128:(i + 1) * 128], wb.ap[:, kc, :]) for kc in range(16)], reads=[concatT, wb])
                    k.op(DVE, lambda: nc.vector.tensor_tensor(out=tm.ap[:], in0=pst.ap[:], in1=g1b.ap[:, nb * 512:(nb + 1) * 512], op=ALU.mult), reads=[pst, g1b], writes=[tm])
                    k.op(DVE, lambda: nc.vector.tensor_tensor(out=x1[i].ap[:, nb * 512:(nb + 1) * 512], in0=x1[i].ap[:, nb * 512:(nb + 1) * 512], in1=tm.ap[:], op=ALU.add), reads=[tm, x1[i]], writes=[x1[i]])
            k.barrier()
        concat_cm.__exit__(None, None, None)

        selb = Tl(sb("selb", [128, 8, NE], BF16), "selb")
        Pm = Tl(sb("Pm", [128, 8, NE], F32), "Pm")
        Gall = Tl(sb("Gall", [128, 8, NE], F32), "Gall")
        GHL = Tl(sb("GHL", [128, 8, NE, 2], BF16), "GHL")
        with contextlib.ExitStack() as cs:
            def sbc(name, shape, dtype):
                return Tl(cs.enter_context(nc.sbuf_tensor(name, shape, dtype)), name)
            A2b = sbc("A2b", [128, D], F32); B2b = sbc("B2b", [128, D], F32)
            n2b = sbc("n2b", [128, D], F32)
            h2f = sbc("h2f", [128, D], F32)
            h2T = sbc("h2T", [128, 16, 128], F32)
            wr = sbc("wr", [128, 16, NE], F32)
            brb = sbc("brb", [128, NE], F32)
            junkF = sbc("junkF", [128, D], BF16)
            ssF = [sbc(f"ssF{i}", [128, 4], F32) for i in range(2)]
            lg = sbc("lg", [128, NE], F32); m8 = sbc("m8", [128, 8], F32); nm = sbc("nm", [128, 2], F32)
            selF = sbc("selF", [128, NE], F32); ex = sbc("ex", [128, NE], F32); gsum = sbc("gsum", [128, 2], F32)
            ghi = sbc("ghi", [128, NE], F32)
            k.dma(SP, A2b.ap[:], bcast_rows(mod_d.tensor, 4 * D, D), reads=[mod_dT], writes=[A2b])
            k.dma(SP, B2b.ap[:], bcast_rows(mod_d.tensor, 3 * D, D), reads=[mod_dT], writes=[B2b])
            k.dma(SP, n2b.ap[:], bcast_rows(norm2_w.tensor, 0, D), writes=[n2b])
            k.dma(SP, wr.ap[:], w_router.rearrange("(c p) e -> p c e", p=128), writes=[wr])
            k.dma(SP, brb.ap[:], bcast_rows(b_router.tensor, 0, NE), writes=[brb])
            k.op(DVE, lambda: nc.vector.scalar_tensor_tensor(out=A2b.ap[:], in0=A2b.ap[:], scalar=1.0, in1=n2b.ap[:], op0=ALU.add, op1=ALU.mult), reads=[A2b, n2b], writes=[A2b])
            for i in range(8):
                ss = ssF[i % 2]
                rms_rstd(x1[i], x1[i].ap, junkF.ap[:], ss, 0, D)
                k.dma(SP, x1_d[i * 128:(i + 1) * 128, :], x1[i].ap, reads=[x1[i]], writes=[x1_dT])
                k.op(DVE, lambda: nc.vector.scalar_tensor_tensor(out=h2f.ap[:], in0=x1[i].ap, scalar=ss.ap[:, 2:3], in1=A2b.ap[:], op0=ALU.mult, op1=ALU.mult), reads=[x1[i], ss, A2b], writes=[h2f])
                k.op(DVE, lambda: nc.vector.tensor_tensor(out=h2f.ap[:], in0=h2f.ap[:], in1=B2b.ap[:], op=ALU.add), reads=[h2f, B2b], writes=[h2f])
                k.op(ACT, lambda: nc.scalar.copy(out=h2b.ap[:, i, :], in_=h2f.ap[:]), reads=[h2f], writes=[h2b])
                for g in range(4):
                    pst = ps[g]
                    k.deps(PE, [h2f, ident_f], [pst])
                    ins = None
                    for j in range(4):
                        c = 4 * g + j
                        ins = nc.tensor.transpose(out=pst.ap[:, j * 128:(j + 1) * 128], in_=h2f.ap[:, c * 128:(c + 1) * 128], identity=ident_f.ap[:])
                    PE.n += 1; ins.then_inc(PE.sem, 1); tok = (PE.sem, PE.n, PE); k.finish(tok, [h2f, ident_f], [pst])
                    k.op(ACT, lambda: nc.scalar.copy(out=h2T.ap[:, 4 * g:4 * g + 4, :], in_=pst.ap[:].rearrange("p (c t) -> p c t", c=4)), reads=[pst], writes=[h2T])
                k.mmg(ps[4], ps[4].ap[:, 0:NE], [(h2T.ap[:, kc, :], wr.ap[:, kc, :]) for kc in range(16)], reads=[h2T, wr])
                k.op(DVE, lambda: nc.vector.tensor_tensor(out=lg.ap[:], in0=ps[4].ap[:, 0:NE], in1=brb.ap[:], op=ALU.add), reads=[ps[4], brb], writes=[lg])
                k.op(DVE, lambda: nc.vector.max(out=m8.ap[:], in_=lg.ap[:]), reads=[lg], writes=[m8])
                k.op(DVE, lambda: nc.vector.tensor_scalar(out=selF.ap[:], in0=lg.ap[:], scalar1=m8.ap[:, 3:4], scalar2=None, op0=ALU.is_ge), reads=[lg, m8], writes=[selF])
                k.op(DVE, lambda: nc.vector.tensor_scalar(out=nm.ap[:, 0:1], in0=m8.ap[:, 0:1], scalar1=-1.0, scalar2=None, op0=ALU.mult), reads=[m8], writes=[nm])
                k.op(ACT, lambda: nc.scalar.activation(out=ex.ap[:], in_=lg.ap[:], func=AF.Exp, bias=nm.ap[:, 0:1], scale=1.0), reads=[lg, nm], writes=[ex])
                k.op(DVE, lambda: nc.vector.tensor_tensor(out=ex.ap[:], in0=ex.ap[:], in1=selF.ap[:], op=ALU.mult), reads=[ex, selF], writes=[ex])
                k.op(DVE, lambda: nc.vector.reduce_sum(out=gsum.ap[:, 0:1], in_=ex.ap[:], axis=mybir.AxisListType.X), reads=[ex], writes=[gsum])
                k.op(DVE, lambda: nc.vector.reciprocal(out=gsum.ap[:, 1:2], in_=gsum.ap[:, 0:1]), reads=[gsum], writes=[gsum])
                k.op(DVE, lambda: nc.vector.tensor_scalar(out=Gall.ap[:, i, :], in0=ex.ap[:], scalar1=gsum.ap[:, 1:2], scalar2=None, op0=ALU.mult), reads=[ex, gsum], writes=[Gall])
                k.op(DVE, lambda: nc.vector.tensor_copy(out=selb.ap[:, i, :], in_=selF.ap[:]), reads=[selF], writes=[selb])
                k.op(DVE, lambda: nc.vector.tensor_copy(out=GHL.ap[:, i, :, 0], in_=Gall.ap[:, i, :]), reads=[Gall], writes=[GHL])
                k.op(DVE, lambda: nc.vector.tensor_copy(out=ghi.ap[:], in_=GHL.ap[:, i, :, 0]), reads=[GHL], writes=[ghi])
                k.op(DVE, lambda: nc.vector.tensor_tensor(out=ghi.ap[:], in0=Gall.ap[:, i, :], in1=ghi.ap[:], op=ALU.subtract), reads=[Gall, ghi], writes=[ghi])
                k.op(DVE, lambda: nc.vector.tensor_copy(out=GHL.ap[:, i, :, 1], in_=ghi.ap[:]), reads=[ghi], writes=[GHL])
                ops_ = [(ones_b.ap[:], selb.ap[:, i2, :]) for i2 in range(i)] + [(strict_b.ap[:], selb.ap[:, i, :])]
                k.mmg(ps[5], ps[5].ap[:, 0:NE], ops_, reads=[ones_b, strict_b, selb])
                k.op(DVE, lambda: nc.vector.scalar_tensor_tensor(out=Pm.ap[:, i, :], in0=ps[5].ap[:, 0:NE], scalar=1.0, in1=selF.ap[:], op0=ALU.add, op1=ALU.mult), reads=[ps[5], selF], writes=[Pm])
                k.op(DVE, lambda: nc.vector.tensor_scalar(out=Pm.ap[:, i, :], in0=Pm.ap[:, i, :], scalar1=-1.0, scalar2=None, op0=ALU.add), reads=[Pm], writes=[Pm])
            k.barrier()

        yacc = x1
        for i in range(8):
            k.op(DVE, lambda: nc.vector.memset(yacc[i].ap, 0.0), writes=[yacc[i]])

        with contextlib.ExitStack() as cs:
            def sbc(name, shape, dtype):
                return Tl(cs.enter_context(nc.sbuf_tensor(name, shape, dtype)), name)
            binT = sbc("binT", [128, NE * 32], F32)
            Pe = [sbc("Pe0", [128, 8, CAP], BF16)] * 2
            PT = sbc("PT", [128, 3, NT], BF16)
            XT = [sbc("XT0", [128, 16, CAP], BF16)] * 2
            HT = sbc("HT", [128, 16, CAP], BF16)
            Ygs = [sbc(f"Yg{i}", [128, 3, 512], BF16) for i in range(2)]
            gs = sbc("gs", [128, 4], F32)
            gcb = [sbc(f"gc{i}", [128, CAP], F32) for i in range(2)]
            sgb = [sbc("sg0", [128, CAP], F32)] * 2
            lcb = [sbc("lc0", [128, CAP], F32)] * 2
            stage = gcb[0]
            for r in range(8):
                k.dma(SP, stage.ap[:, 0:128], b_exp_in[r * 128:(r + 1) * 128, :], writes=[stage])
                k.deps(PE, [stage, ident_f], [ps[0]])
                ins = nc.tensor.transpose(out=ps[0].ap[:, 0:128], in_=stage.ap[:, 0:128], identity=ident_f.ap[:])
                PE.n += 1; ins.then_inc(PE.sem, 1); tok = (PE.sem, PE.n, PE); k.finish(tok, [stage, ident_f], [ps[0]])
                k.op(DVE, lambda: nc.vector.tensor_copy(out=binT.ap[:, r * 128:(r + 1) * 128], in_=ps[0].ap[:, 0:128]), reads=[ps[0]], writes=[binT])
            psb = Tl(ps[7].ap[:].bitcast(BF16), "psbf")
            SC = [(0, 128), (128, 128), (256, CAP - 256)]
            wq = []
            wcount = [0]

            def load_block(src_ap):
                wb = wbuf[wcount[0] % 3]; wcount[0] += 1
                k.dma(POOL, wb.ap[:], src_ap, writes=[wb])
                return wb

            w_in_e = w_exp_in.rearrange("e (c p) f -> e p c f", p=128)
            w_out_e = w_exp_out.rearrange("e (c p) f -> e p c f", p=128)

            def block_list(e):
                bl = []
                for j in range(4):
                    bl.append(("g", j, w_in_e[e, :, :, j * 512:(j + 1) * 512]))
                    bl.append(("l", j, w_in_e[e, :, :, D + j * 512:D + (j + 1) * 512]))
                for j in range(4):
                    bl.append(("o", j, w_out_e[e, :, :, j * 512:(j + 1) * 512]))
                return bl

            allblocks = []
            for e in range(N_EXPERTS_RUN):
                allblocks += [(e,) + b for b in block_list(e)]
            PREF = 2
            loaded = []
            for bi in range(min(PREF, len(allblocks))):
                loaded.append(load_block(allblocks[bi][3]))
            nloaded = [len(loaded)]

            def next_block(bi):
                nb_ = bi + PREF
                if nb_ < len(allblocks) and nloaded[0] <= nb_:
                    loaded.append(load_block(allblocks[nb_][3])); nloaded[0] += 1
                return loaded[bi]

            bi = 0
            upc = 0
            for e in range(N_EXPERTS_RUN):
                pe_t = Pe[e % 2]; xt_t = XT[e % 2]
                for i in range(8):
                    k.op(DVE, lambda: nc.vector.tensor_scalar(out=pe_t.ap[:, i, :], in0=iota_c.ap[:], scalar1=Pm.ap[:, i, e:e + 1], scalar2=None, op0=ALU.is_equal), reads=[iota_c, Pm], writes=[pe_t])
                for sc, (s0, sn) in enumerate(SC):
                    k.deps(PE, [pe_t, ident_b], [ps[7]])
                    ins = None
                    for i in range(8):
                        ins = nc.tensor.transpose(out=psb.ap[0:sn, i * 128:(i + 1) * 128], in_=pe_t.ap[:, i, s0:s0 + sn], identity=ident_b.ap[:])
                    PE.n += 1; ins.then_inc(PE.sem, 1); tok = (PE.sem, PE.n, PE); k.finish(tok, [pe_t, ident_b], [ps[7]])
                    k.op(ACT, lambda: nc.scalar.copy(out=PT.ap[0:sn, sc, :], in_=psb.ap[0:sn, :]), reads=[ps[7]], writes=[PT])
                    k.mmg(ps[6], ps[6].ap[0:sn, 0:2], [(pe_t.ap[:, i, s0:s0 + sn], GHL.ap[:, i, e, :]) for i in range(8)], reads=[pe_t, GHL])
                    k.op(DVE, lambda: nc.vector.reduce_sum(out=gs.ap[0:sn, sc:sc + 1], in_=ps[6].ap[0:sn, 0:2], axis=mybir.AxisListType.X), reads=[ps[6]], writes=[gs])
                for kc in range(16):
                    pst = ps[kc % 2]
                    k.mmg(pst, pst.ap[:, 0:CAP], [(h2b.ap[:, i, kc * 128:(kc + 1) * 128], pe_t.ap[:, i, :]) for i in range(8)], reads=[h2b, pe_t])
                    if kc % 2 == 0:
                        k.op(ACT, lambda: nc.scalar.copy(out=xt_t.ap[:, kc, :], in_=pst.ap[:, 0:CAP]), reads=[pst], writes=[xt_t])
                    else:
                        k.op(DVE, lambda: nc.vector.tensor_copy(out=xt_t.ap[:, kc, :], in_=pst.ap[:, 0:CAP]), reads=[pst], writes=[xt_t])
                for j in range(4):
                    wg = next_block(bi); bi += 1
                    for c in range(4):
                        fc = 4 * j + c
                        pst = ps[2 + upc % 2]; gc = gcb[upc % 2]; sg = sgb[upc % 2]; upc += 1
                        k.mmg(pst, pst.ap[:, 0:CAP], [(wg.ap[:, kc, c * 128:(c + 1) * 128], xt_t.ap[:, kc, :]) for kc in range(16)], reads=[wg, xt_t])
                        bcol = e * 32 + fc
                        k.op(DVE, lambda: nc.vector.tensor_scalar(out=gc.ap[:], in0=pst.ap[:, 0:CAP], scalar1=binT.ap[:, bcol:bcol + 1], scalar2=7.0, op0=ALU.add, op1=ALU.min), reads=[pst, binT], writes=[gc])
                        k.op(ACT, lambda: nc.scalar.activation(out=sg.ap[:], in_=gc.ap[:], func=AF.Sigmoid, scale=1.702), reads=[gc], writes=[sg])
                        k.op(DVE, lambda: nc.vector.tensor_tensor(out=HT.ap[:, fc, :], in0=gc.ap[:], in1=sg.ap[:], op=ALU.mult), reads=[gc, sg], writes=[HT])
                    wl = next_block(bi); bi += 1
                    for c in range(4):
                        fc = 4 * j + c
                        pst = ps[2 + upc % 2]; lc = lcb[upc % 2]; upc += 1
                        k.mmg(pst, pst.ap[:, 0:CAP], [(wl.ap[:, kc, c * 128:(c + 1) * 128], xt_t.ap[:, kc, :]) for kc in range(16)], reads=[wl, xt_t])
                        bcol = e * 32 + 16 + fc
                        k.op(DVE, lambda: nc.vector.tensor_scalar(out=lc.ap[:], in0=pst.ap[:, 0:CAP], scalar1=binT.ap[:, bcol:bcol + 1], scalar2=-7.0, op0=ALU.add, op1=ALU.max), reads=[pst, binT], writes=[lc])
                        k.op(DVE, lambda: nc.vector.tensor_scalar(out=lc.ap[:], in0=lc.ap[:], scalar1=7.0, scalar2=1.0, op0=ALU.min, op1=ALU.add), reads=[lc], writes=[lc])
                        k.op(DVE, lambda: nc.vector.tensor_tensor(out=HT.ap[:, fc, :], in0=HT.ap[:, fc, :], in1=lc.ap[:], op=ALU.mult), reads=[HT, lc], writes=[HT])
                def down(j):
                    wo_ = next_block(bi_box[0]); bi_box[0] += 1
                    Yg = Ygs[j % 2]
                    for sc, (s0, sn) in enumerate(SC):
                        pst = ps[4 + (3 * j + sc) % 2]
                        k.mmg(pst, pst.ap[0:sn, :], [(HT.ap[:, fcc, s0:s0 + sn], wo_.ap[:, fcc, :]) for fcc in range(16)], reads=[HT, wo_])
                        k.op(ACT, lambda: nc.scalar.activation(out=Yg.ap[0:sn, sc, :], in_=pst.ap[0:sn, :], func=AF.Identity, scale=gs.ap[0:sn, sc:sc + 1]), reads=[pst, gs], writes=[Yg])

                def scatter(j):
                    Yg = Ygs[j % 2]
                    for i in range(8):
                        pst = ps[i % 2]
                        k.mmg(pst, pst.ap[:], [(PT.ap[0:sn, sc, i * 128:(i + 1) * 128], Yg.ap[0:sn, sc, :]) for sc, (s0, sn) in enumerate(SC)], reads=[PT, Yg])
                        k.op(DVE, lambda: nc.vector.tensor_tensor(out=yacc[i].ap[:, j * 512:(j + 1) * 512], in0=yacc[i].ap[:, j * 512:(j + 1) * 512], in1=pst.ap[:], op=ALU.add), reads=[pst, yacc[i]], writes=[yacc[i]])
                bi_box = [bi]
                down(0)
                for j in range(4):
                    if j + 1 < 4:
                        down(j + 1)
                    scatter(j)
                bi = bi_box[0]
            k.barrier()

        with contextlib.ExitStack() as cs:
            def sbc(name, shape, dtype):
                return Tl(cs.enter_context(nc.sbuf_tensor(name, shape, dtype)), name)
            g2b = sbc("g2b", [128, D], F32)
            bo = sbc("bo", [NE, D], F32)
            xr = [sbc(f"xr{i}", [128, D], F32) for i in range(2)]
            k.dma(SP, g2b.ap[:], bcast_rows(mod_d.tensor, 5 * D, D), reads=[mod_dT], writes=[g2b])
            k.dma(SP, bo.ap[:], b_exp_out[:, :], writes=[bo])
            GT = sbc("GT", [NE, NT], F32)
            for i in range(8):
                k.deps(PE, [Gall, ident_f], [ps[6]])
                ins = nc.tensor.transpose(out=ps[6].ap[0:NE, 0:128], in_=Gall.ap[:, i, :], identity=ident_f.ap[:])
                PE.n += 1; ins.then_inc(PE.sem, 1); tok = (PE.sem, PE.n, PE); k.finish(tok, [Gall, ident_f], [ps[6]])
                k.op(ACT, lambda: nc.scalar.copy(out=GT.ap[:, i * 128:(i + 1) * 128], in_=ps[6].ap[0:NE, 0:128]), reads=[ps[6]], writes=[GT])
            cc_ = 0
            for i in range(8):
                xt = xr[i % 2]
                k.dma(SP, xt.ap[:], x1_d[i * 128:(i + 1) * 128, :], reads=[x1_dT], writes=[xt])
                for nb in range(4):
                    pst = ps[cc_ % 2]; cc_ += 1
                    k.mmg(pst, pst.ap[:], [(GT.ap[:, i * 128:(i + 1) * 128], bo.ap[:, nb * 512:(nb + 1) * 512])], reads=[GT, bo])
                    sl = slice(nb * 512, (nb + 1) * 512)
                    k.op(DVE, lambda: nc.vector.tensor_tensor(out=yacc[i].ap[:, sl], in0=yacc[i].ap[:, sl], in1=pst.ap[:], op=ALU.add), reads=[pst, yacc[i]], writes=[yacc[i]])
                k.op(DVE, lambda: nc.vector.tensor_tensor(out=yacc[i].ap, in0=yacc[i].ap, in1=g2b.ap[:], op=ALU.mult), reads=[yacc[i], g2b], writes=[yacc[i]])
                k.op(DVE, lambda: nc.vector.tensor_tensor(out=xt.ap[:], in0=xt.ap[:], in1=yacc[i].ap, op=ALU.add), reads=[yacc[i], xt], writes=[xt])
                k.dma(SP, out[i * 128:(i + 1) * 128, :], xt.ap[:], reads=[xt], writes=[outT])
            k.wait(SP, (outT.dsem, outT.dcnt, None))
            if DEBUG:
                k.wait(SP, (x1_dT.dsem, x1_dT.dcnt, None))

    return nc


def make_in_maps(inputs, ne_w=NE):
    f = lambda a: np.ascontiguousarray(np.asarray(a, dtype=np.float32))
    x = f(inputs["x"]); c = f(inputs["c"])
    shared = {
        "norm1_w": f(np.asarray(inputs["norm1_w"])[0].reshape(16, 128).T),
        "norm2_w": f(np.asarray(inputs["norm2_w"])[0]),
        "w_ada": f(np.asarray(inputs["w_ada"])[0]),
        "b_ada": f(np.asarray(inputs["b_ada"])[0]),
        "w_in": f(np.asarray(inputs["w_in"])[0]),
        "qk_w": f(np.stack([np.asarray(inputs["q_norm_w"])[0], np.asarray(inputs["k_norm_w"])[0]], axis=1)),
        "w_pool": f(np.asarray(inputs["w_pool"])[0]),
        "pool_scale": f(np.asarray(inputs["pool_scale"])[0].reshape(8, 128).T),
        "w_o": f(np.asarray(inputs["w_o"])[0]),
        "w_router": f(np.asarray(inputs["w_router"])[0]),
        "b_router": f(np.asarray(inputs["b_router"])[0]),
        "w_exp_in": f(np.asarray(inputs["w_exp_in"])[0][:ne_w]),
        "b_exp_in": f(np.asarray(inputs["b_exp_in"])[0].reshape(NE * 32, 128)),
        "w_exp_out": f(np.asarray(inputs["w_exp_out"])[0][:ne_w]),
        "b_exp_out": f(np.asarray(inputs["b_exp_out"])[0]),
    }
    in_maps = []
    for core in range(8):
        b, half = core // 2, core % 2
        fl = np.zeros((128, 2), np.float32)
        fl[:, 0] = 0.0 if half == 1 else NEGB
        fl[:, 1] = 1.0 if half == 1 else 0.0
        m = dict(shared)
        m["x_own"] = f(x[b, half * NT:(half + 1) * NT])
        m["x_prev"] = f(x[b, 0:NT])
        m["c_own"] = f(c[b].reshape(16, 128).T)
        m["flags"] = fl
        in_maps.append(m)
    return in_maps


_NC_CACHE = {}


def kernel(**inputs):
    in_maps = make_in_maps(inputs)
    if "nc" not in _NC_CACHE:
        _NC_CACHE["nc"] = build_nc()
    nc = _NC_CACHE["nc"]
    res = run_bass_kernel_spmd(nc, in_maps, core_ids=list(range(8)))
    outp = np.zeros((4, 2048, D), np.float32)
    for core in range(8):
        b, half = core // 2, core % 2
        outp[b, half * NT:(half + 1) * NT] = res.results[core]["out"]
    return outp
```

```python
import contextlib
import numpy as np
import concourse.bass as bass
import concourse.mybir as mybir
from concourse.bass_utils import run_bass_kernel_spmd

F32 = mybir.dt.float32
BF16 = mybir.dt.bfloat16
I32 = mybir.dt.int32
AF = mybir.ActivationFunctionType
ALU = mybir.AluOpType

D = 2048
NT = 1024
NE = 32
CAP = 352
EPS = 1e-6
NEGB = -30000.0


class Tl:
    __slots__ = ("ap", "w", "r", "dsem", "dcnt", "name")

    def __init__(s, ap, name=""):
        s.ap = ap; s.w = None; s.r = {}; s.dsem = None; s.dcnt = 0; s.name = name


class Eng:
    def __init__(s, k, name, eng, is_pe=False):
        s.eng = eng; s.sem = k.newsem(name); s.n = 0; s.waited = {}; s.is_pe = is_pe; s.name = name


class KB:
    def __init__(s, nc, es):
        s.nc = nc; s.es = es; s.nsem = 0; s.dsems = []
        s.pe = Eng(s, "pe", nc.tensor, True)
        s.act = Eng(s, "act", nc.scalar)
        s.dve = Eng(s, "dve", nc.vector)
        s.pool = Eng(s, "pool", nc.gpsimd)
        s.sp = Eng(s, "sp", nc.sync)
        s.engs = [s.pe, s.act, s.dve, s.pool, s.sp]

    def newsem(s, name):
        s.nsem += 1
        return s.es.enter_context(s.nc.semaphore(f"{name}_{s.nsem}"))

    def wait(s, e, tok):
        sem, val, src = tok
        if src is e:
            if e.is_pe or e.n - val >= 3:
                return
        key = id(sem)
        if e.waited.get(key, 0) >= val:
            return
        e.eng.wait_ge(sem, val)
        e.waited[key] = val

    def deps(s, e, reads, writes, dma_sem=None):
        for t in reads:
            if t.w is not None:
                s.wait(e, t.w)
        for t in writes:
            if t.w is not None and not (dma_sem is not None and t.w[0] is dma_sem):
                s.wait(e, t.w)
            for tok in t.r.values():
                s.wait(e, tok)

    def finish(s, tok, reads, writes):
        for t in writes:
            t.w = tok; t.r = {}
        for t in reads:
            key = id(tok[0]); old = t.r.get(key)
            if old is None or old[1] < tok[1]:
                t.r[key] = tok

    def op(s, e, fn, reads=(), writes=()):
        s.deps(e, reads, writes)
        ins = fn()
        e.n += 1
        ins.then_inc(e.sem, 1)
        tok = (e.sem, e.n, e)
        s.finish(tok, reads, writes)
        return tok

    def dma(s, e, out, in_, reads=(), writes=()):
        tgt = writes[0]
        if tgt.dsem is None:
            tgt.dsem = s.newsem("d" + tgt.name); s.dsems.append(tgt)
        s.deps(e, reads, writes, dma_sem=tgt.dsem)
        ins = e.eng.dma_start(out=out, in_=in_)
        tgt.dcnt += 16
        ins.then_inc(tgt.dsem, 16)
        tok = (tgt.dsem, tgt.dcnt, None)
        s.finish(tok, reads, writes)
        return tok

    def mmg(s, out_t, out_ap, ops, reads):
        nc = s.nc
        s.deps(s.pe, reads, [out_t])
        n = len(ops)
        ins = None
        for i, (l, r) in enumerate(ops):
            ins = nc.tensor.matmul(out=out_ap, lhsT=l, rhs=r, start=(i == 0), stop=(i == n - 1))
        s.pe.n += 1
        ins.then_inc(s.pe.sem, 1)
        tok = (s.pe.sem, s.pe.n, s.pe)
        s.finish(tok, reads, [out_t])
        return tok

    def barrier(s):
        for e in s.engs:
            for f in s.engs:
                if f is not e and f.n > 0:
                    s.wait(e, (f.sem, f.n, f))
            for t in s.dsems:
                if t.dcnt:
                    s.wait(e, (t.dsem, t.dcnt, None))


def bcast_rows(ap1d_tensor, offset, n, parts=128):
    return bass.AP(ap1d_tensor, offset, [[0, parts], [1, n]])


def build_nc(ne_w=NE, DEBUG=False):
    N_EXPERTS_RUN = ne_w
    nc = bass.Bass("TRN2", target_bir_lowering=False)
    dt = nc.dram_tensor
    x_own = dt("x_own", [NT, D], F32, kind="ExternalInput").ap()
    x_prev = dt("x_prev", [NT, D], F32, kind="ExternalInput").ap()
    c_own = dt("c_own", [128, 16], F32, kind="ExternalInput").ap()
    flags = dt("flags", [128, 2], F32, kind="ExternalInput").ap()
    norm1_w = dt("norm1_w", [128, 16], F32, kind="ExternalInput").ap()
    norm2_w = dt("norm2_w", [D], F32, kind="ExternalInput").ap()
    w_ada = dt("w_ada", [D, 6 * D], F32, kind="ExternalInput").ap()
    b_ada = dt("b_ada", [6 * D], F32, kind="ExternalInput").ap()
    w_in = dt("w_in", [D, 2 * D], F32, kind="ExternalInput").ap()
    qk_w = dt("qk_w", [128, 2], F32, kind="ExternalInput").ap()
    w_pool = dt("w_pool", [4, 256, 256], F32, kind="ExternalInput").ap()
    pool_scale = dt("pool_scale", [128, 8], F32, kind="ExternalInput").ap()
    w_o = dt("w_o", [D, D], F32, kind="ExternalInput").ap()
    w_router = dt("w_router", [D, NE], F32, kind="ExternalInput").ap()
    b_router = dt("b_router", [NE], F32, kind="ExternalInput").ap()
    w_exp_in = dt("w_exp_in", [ne_w, D, 2 * D], F32, kind="ExternalInput").ap()
    b_exp_in = dt("b_exp_in", [NE * 32, 128], F32, kind="ExternalInput").ap()
    w_exp_out = dt("w_exp_out", [ne_w, D, D], F32, kind="ExternalInput").ap()
    b_exp_out = dt("b_exp_out", [NE, D], F32, kind="ExternalInput").ap()
    out = dt("out", [NT, D], F32, kind="ExternalOutput").ap()
    mod_d = dt("mod_d", [6 * D], F32, kind="Internal").ap()
    x1_d = dt("x1_d", [NT, D], F32, kind="ExternalOutput" if DEBUG else "Internal").ap()

    with contextlib.ExitStack() as es:
        k = KB(nc, es)
        PE, ACT, DVE, POOL, SP = k.pe, k.act, k.dve, k.pool, k.sp

        def sb(name, shape, dtype):
            return es.enter_context(nc.sbuf_tensor(name, shape, dtype))

        wbuf = [Tl(sb(f"wbuf{i}", [128, 16, 512], BF16), f"wbuf{i}") for i in range(3)]
        big = sb("big", [128, 24576], F32)
        ident_f = Tl(sb("ident_f", [128, 128], F32), "identf")
        ident_b = Tl(sb("ident_b", [128, 128], BF16), "identb")
        tri_b = Tl(sb("tri_b", [128, 128], BF16), "tri")
        ones_b = Tl(sb("ones_b", [128, 128], BF16), "ones")
        strict_b = Tl(sb("strict_b", [128, 128], BF16), "strict")
        iota_c = Tl(sb("iota_c", [128, CAP], F32), "iotac")
        small = Tl(sb("small", [128, 64], F32), "small")
        modT = Tl(sb("modT", [128, 96], F32), "modT")
        flg = Tl(sb("flg", [128, 2], F32), "flg")
        ps = [Tl(es.enter_context(nc.psum_tensor(f"ps{i}", [128, 512], F32)), f"ps{i}") for i in range(8)]
        mod_dT = Tl(mod_d, "mod_d")
        x1_dT = Tl(x1_d, "x1_d")
        outT = Tl(out, "out")

        ZC = small.ap[:, 0:1]

        with contextlib.ExitStack() as cs:
            ii = Tl(cs.enter_context(nc.sbuf_tensor("ii", [128, 512], I32)), "ii")
            ff = Tl(cs.enter_context(nc.sbuf_tensor("ff", [128, 512], F32)), "ff")
            k.op(POOL, lambda: nc.gpsimd.iota(ii.ap[:, 0:128], pattern=[[1, 128]], base=0, channel_multiplier=-1), writes=[ii])
            k.op(DVE, lambda: nc.vector.tensor_copy(out=ff.ap[:, 0:128], in_=ii.ap[:, 0:128]), reads=[ii], writes=[ff])
            k.op(DVE, lambda: nc.vector.tensor_scalar(out=ident_f.ap[:], in0=ff.ap[:, 0:128], scalar1=0.0, scalar2=None, op0=ALU.is_equal), reads=[ff], writes=[ident_f])
            k.op(DVE, lambda: nc.vector.tensor_scalar(out=ident_b.ap[:], in0=ff.ap[:, 0:128], scalar1=0.0, scalar2=None, op0=ALU.is_equal), reads=[ff], writes=[ident_b])
            k.op(DVE, lambda: nc.vector.tensor_scalar(out=tri_b.ap[:], in0=ff.ap[:, 0:128], scalar1=0.0, scalar2=None, op0=ALU.is_le), reads=[ff], writes=[tri_b])
            k.op(DVE, lambda: nc.vector.tensor_scalar(out=strict_b.ap[:], in0=ff.ap[:, 0:128], scalar1=0.0, scalar2=None, op0=ALU.is_gt), reads=[ff], writes=[strict_b])
            k.op(DVE, lambda: nc.vector.memset(ones_b.ap[:], 1.0), writes=[ones_b])
            k.op(DVE, lambda: nc.vector.memset(small.ap[:], 0.0), writes=[small])
            k.op(POOL, lambda: nc.gpsimd.iota(ii.ap[:, 0:CAP], pattern=[[1, CAP]], base=0, channel_multiplier=0), reads=[], writes=[ii])
            k.op(DVE, lambda: nc.vector.tensor_copy(out=iota_c.ap[:], in_=ii.ap[:, 0:CAP]), reads=[ii], writes=[iota_c])
            k.dma(SP, flg.ap[:], flags[:, :], writes=[flg])
            k.barrier()

        cTb = Tl(sb("cTb", [128, 16], BF16), "cTb")
        stg = Tl(sb("stg", [1, 512], F32), "stg")
        bst = Tl(sb("bst", [1, 512], F32), "bst")
        wav = w_ada.rearrange("(c p) f -> p c f", p=128)

        def mod_load(nb, wb):
            k.dma(POOL, wb.ap[:], wav[:, :, nb * 512:(nb + 1) * 512], writes=[wb])

        def mod_block(nb, wb, pst, load=True):
            k.dma(SP, bst.ap[:], bass.AP(b_ada.tensor, nb * 512, [[0, 1], [1, 512]]), writes=[bst])
            if load:
                mod_load(nb, wb)
            k.mmg(pst, pst.ap[0:1, :], [(cTb.ap[:, kc:kc + 1], wb.ap[:, kc, :]) for kc in range(16)], reads=[cTb, wb])
            k.op(DVE, lambda: nc.vector.tensor_tensor(out=stg.ap[:], in0=pst.ap[0:1, :], in1=bst.ap[:], op=ALU.add), reads=[pst, bst], writes=[stg])
            k.dma(SP, bass.AP(mod_d.tensor, nb * 512, [[0, 1], [1, 512]]), stg.ap[:], reads=[stg], writes=[mod_dT])

        with contextlib.ExitStack() as cs:
            cT = Tl(cs.enter_context(nc.sbuf_tensor("cT", [128, 16], F32)), "cT")
            k.dma(SP, cT.ap[:], c_own[:, :], writes=[cT])
            k.op(ACT, lambda: nc.scalar.activation(out=cTb.ap[:], in_=cT.ap[:], func=AF.Silu), reads=[cT], writes=[cTb])
            for nb in range(8):
                mod_block(nb, wbuf[nb % 3], ps[nb % 2])
            with nc.allow_non_contiguous_dma(reason="small one-time relayout of the modulation vector"):
                k.dma(SP, modT.ap[:, 0:32], bass.AP(mod_d.tensor, 0, [[1, 128], [128, 32]]), reads=[mod_dT], writes=[modT])
            k.barrier()

        A1T = Tl(sb("A1T", [128, 16], F32), "A1T")
        n1w = Tl(sb("n1w", [128, 16], F32), "n1w")
        k.dma(SP, n1w.ap[:], norm1_w[:, :], writes=[n1w])
        k.op(DVE, lambda: nc.vector.scalar_tensor_tensor(out=A1T.ap[:], in0=modT.ap[:, 16:32], scalar=1.0, in1=n1w.ap[:], op0=ALU.add, op1=ALU.mult), reads=[modT, n1w], writes=[A1T])
        qkw = Tl(sb("qkw", [128, 2], F32), "qkw")
        k.dma(SP, qkw.ap[:], qk_w[:, :], writes=[qkw])
        k.op(DVE, lambda: nc.vector.tensor_scalar(out=small.ap[:, 2:3], in0=qkw.ap[:, 0:1], scalar1=float(128 ** -0.5), scalar2=None, op0=ALU.mult), reads=[qkw], writes=[small])
        k.op(DVE, lambda: nc.vector.tensor_copy(out=small.ap[:, 3:4], in_=qkw.ap[:, 1:2]), reads=[qkw], writes=[small])

        hT_ap = big[:, 0:16384].bitcast(BF16).rearrange("p (c t) -> p c t", c=16)
        hT = Tl(hT_ap, "hT")
        spare = big[:, 16384:24576]

        def rms_rstd(src_t, src_ap, junk_ap, ss_t, col, n):
            k.op(DVE, lambda: nc.vector.memset(ss_t.ap[:, col:col + 1], 0.0), writes=[ss_t])
            k.op(ACT, lambda: nc.scalar.activation(out=junk_ap, in_=src_ap, func=AF.Square, accum_out=ss_t.ap[:, col:col + 1]), reads=[src_t, ss_t], writes=[ss_t])
            k.op(DVE, lambda: nc.vector.tensor_scalar(out=ss_t.ap[:, col + 1:col + 2], in0=ss_t.ap[:, col:col + 1], scalar1=1.0 / n, scalar2=EPS, op0=ALU.mult, op1=ALU.add), reads=[ss_t], writes=[ss_t])
            k.op(ACT, lambda: nc.scalar.activation(out=ss_t.ap[:, col + 1:col + 2], in_=ss_t.ap[:, col + 1:col + 2], func=AF.Sqrt), reads=[ss_t], writes=[ss_t])
            k.op(DVE, lambda: nc.vector.reciprocal(out=ss_t.ap[:, col + 2:col + 3], in_=ss_t.ap[:, col + 1:col + 2]), reads=[ss_t], writes=[ss_t])

        concat_cm = nc.sbuf_tensor("concatT", [128, 16, NT], BF16)
        concatT = Tl(concat_cm.__enter__(), "concatT")

        with contextlib.ExitStack() as cs:
            xts = [Tl(spare[:, i * 2048:(i + 1) * 2048], f"xt{i}") for i in range(2)]
            junk = Tl(spare[:, 4096:5120].bitcast(BF16), "junk")
            sst = [Tl(cs.enter_context(nc.sbuf_tensor(f"ssB{i}", [128, 4], F32)), f"ssB{i}") for i in range(2)]
            for ti in range(16):
                src = x_prev if ti < 8 else x_own
                r0 = (ti % 8) * 128
                xt = xts[ti % 2]; ss = sst[ti % 2]
                k.dma(SP, xt.ap, src[r0:r0 + 128, :], writes=[xt])
                rms_rstd(xt, xt.ap, junk.ap, ss, 0, D)
                k.op(DVE, lambda: nc.vector.tensor_scalar(out=xt.ap, in0=xt.ap, scalar1=ss.ap[:, 2:3], scalar2=None, op0=ALU.mult), reads=[xt, ss], writes=[xt])
                for g in range(4):
                    pst = ps[g]
                    k.deps(PE, [xt, ident_f], [pst])
                    ins = None
                    for j in range(4):
                        c = 4 * g + j
                        ins = nc.tensor.transpose(out=pst.ap[:, j * 128:(j + 1) * 128], in_=xt.ap[:, c * 128:(c + 1) * 128], identity=ident_f.ap[:])
                    PE.n += 1; ins.then_inc(PE.sem, 1); tok = (PE.sem, PE.n, PE); k.finish(tok, [xt, ident_f], [pst])
                    for j in range(4):
                        c = 4 * g + j
                        if j % 2 == 0:
                            k.op(ACT, lambda: nc.scalar.activation(out=hT.ap[:, c, ti * 128:(ti + 1) * 128], in_=pst.ap[:, j * 128:(j + 1) * 128], func=AF.Identity, scale=A1T.ap[:, c:c + 1], bias=modT.ap[:, c:c + 1]), reads=[pst, A1T, modT], writes=[hT])
                        else:
                            k.op(DVE, lambda: nc.vector.tensor_scalar(out=hT.ap[:, c, ti * 128:(ti + 1) * 128], in0=pst.ap[:, j * 128:(j + 1) * 128], scalar1=A1T.ap[:, c:c + 1], scalar2=modT.ap[:, c:c + 1], op0=ALU.mult, op1=ALU.add), reads=[pst, A1T, modT], writes=[hT])
            k.barrier()

        w_in_v = w_in.rearrange("(c p) f -> p c f", p=128)
        with contextlib.ExitStack() as cs:
            def sbc(name, shape, dtype):
                return Tl(cs.enter_context(nc.sbuf_tensor(name, shape, dtype)), name)
            NEG = Tl(spare[:, 0:2048].rearrange("p (j t) -> p j t", j=4), "NEG")
            QT = [Tl(spare[:, 2048 + i * 512:2048 + (i + 1) * 512].bitcast(BF16), f"QT{i}") for i in range(2)]
            KT = [Tl(spare[:, 3072 + i * 1024:3072 + (i + 1) * 1024].bitcast(BF16), f"KT{i}") for i in range(2)]
            VV = [Tl(spare[:, 5120 + i * 1024:5120 + (i + 1) * 1024].bitcast(BF16).rearrange("p (t d) -> p t d", t=16), f"V{i}") for i in range(2)]
            sq = [sbc("sq0", [128, 512], BF16)] * 2
            rstd = [sbc("rstd0", [128, 512], F32)] * 2
            vtmp = sbc("vtmp", [128, 512], BF16)
            Eb = [sbc(f"E{i}", [128, 512], F32) for i in range(2)]
            Lb = [sbc(f"L{i}", [128, 512], BF16) for i in range(2)]
            T1 = sbc("T1", [128, 512], F32)
            Zmb = [sbc(f"Zm{i}", [128, 512], F32) for i in range(2)]
            T2 = [sbc("T20", [128, 512], F32)] * 2
            Ab = [sbc(f"A{i}", [128, 512], BF16) for i in range(2)]
            Rb = sbc("Rb", [128, 512], F32)
            ps2b = ps[2].ap[:].bitcast(BF16)
            ii = Tl(T1.ap[:].bitcast(I32), "iiC")
            for j in range(4):
                k.op(POOL, lambda: nc.gpsimd.iota(ii.ap, pattern=[[1, 512]], base=-128 * j, channel_multiplier=-1), writes=[ii])
                k.op(DVE, lambda: nc.vector.tensor_copy(out=NEG.ap[:, j, :], in_=ii.ap), reads=[ii], writes=[NEG])
                k.op(DVE, lambda: nc.vector.tensor_scalar(out=NEG.ap[:, j, :], in0=NEG.ap[:, j, :], scalar1=0.0, scalar2=NEGB, op0=ALU.is_le, op1=ALU.mult), reads=[NEG], writes=[NEG])
            k.barrier()

            def qk_norm(pst, dst_t, dst_ap, wcol):
                sqt = sq[0]; rs = rstd[0]
                k.op(ACT, lambda: nc.scalar.activation(out=sqt.ap[:], in_=pst.ap[:], func=AF.Square), reads=[pst], writes=[sqt])
                k.mmg(ps[2], ps[2].ap[:], [(ones_b.ap[:], sqt.ap[:])], reads=[ones_b, sqt])
                k.op(ACT, lambda: nc.scalar.activation(out=rs.ap[:], in_=ps[2].ap[:], func=AF.Ln, scale=1.0 / 128, bias=small.ap[:, 5:6]), reads=[ps[2], small], writes=[rs])
                k.op(ACT, lambda: nc.scalar.activation(out=rs.ap[:], in_=rs.ap[:], func=AF.Exp, scale=-0.5), reads=[rs], writes=[rs])
                k.op(DVE, lambda: nc.vector.scalar_tensor_tensor(out=dst_ap, in0=pst.ap[:], scalar=small.ap[:, wcol:wcol + 1], in1=rs.ap[:], op0=ALU.mult, op1=ALU.mult), reads=[pst, rs, small], writes=[dst_t])

            k.op(DVE, lambda: nc.vector.memset(small.ap[:, 5:6], EPS), writes=[small])
            acc_ctr = [0]

            def inproj_items(h):
                wb = wbuf[h % 2]; qt = QT[h % 2]; kt = KT[h % 2]; vt = VV[h % 2]
                items = []

                def it_w():
                    for j, c0_ in enumerate((128 * h, 1024 + 128 * h, 2048 + 128 * h)):
                        k.dma(POOL, wb.ap[:, :, j * 128:(j + 1) * 128], w_in_v[:, :, c0_:c0_ + 128], writes=[wb])
                items.append(it_w)

                def mk_q(n_):
                    def f():
                        pst = ps[acc_ctr[0] % 2]; acc_ctr[0] += 1
                        k.mmg(pst, pst.ap[:], [(wb.ap[:, kc, 0:128], hT.ap[:, kc, 1024 + n_ * 512:1024 + (n_ + 1) * 512]) for kc in range(16)], reads=[wb, hT])
                        qk_norm(pst, qt, qt.ap[:, n_ * 512:(n_ + 1) * 512], 2)
                    return f

                def mk_k(n_):
                    def f():
                        pst = ps[acc_ctr[0] % 2]; acc_ctr[0] += 1
                        k.mmg(pst, pst.ap[:], [(wb.ap[:, kc, 128:256], hT.ap[:, kc, n_ * 512:(n_ + 1) * 512]) for kc in range(16)], reads=[wb, hT])
                        qk_norm(pst, kt, kt.ap[:, n_ * 512:(n_ + 1) * 512], 3)
                    return f

                def mk_v(n_):
                    def f():
                        pst = ps[acc_ctr[0] % 2]; acc_ctr[0] += 1
                        k.mmg(pst, pst.ap[:], [(wb.ap[:, kc, 256:384], hT.ap[:, kc, n_ * 512:(n_ + 1) * 512]) for kc in range(16)], reads=[wb, hT])
                        k.op(DVE, lambda: nc.vector.tensor_copy(out=vtmp.ap[:], in_=pst.ap[:]), reads=[pst], writes=[vtmp])
                        k.deps(PE, [vtmp, ident_b], [ps[2]])
                        ins = None
                        for j in range(4):
                            ins = nc.tensor.transpose(out=ps2b[:, j * 128:(j + 1) * 128], in_=vtmp.ap[:, j * 128:(j + 1) * 128], identity=ident_b.ap[:])
                        PE.n += 1; ins.then_inc(PE.sem, 1); tok = (PE.sem, PE.n, PE); k.finish(tok, [vtmp, ident_b], [ps[2]])
                        k.op(DVE, lambda: nc.vector.tensor_copy(out=vt.ap[:, 4 * n_:4 * n_ + 4, :], in_=ps2b[:, 0:512].rearrange("p (t d) -> p t d", t=4)), reads=[ps[2]], writes=[vt])
                    return f
                items += [mk_k(0), mk_q(0), mk_k(1), mk_v(0), mk_k(2), mk_v(1), mk_q(1), mk_k(3), mk_v(2), mk_v(3)]
                return items

            mod_next = [8]

            mod_load(8, wbuf[2])

            def mod_item():
                if mod_next[0] < 24:
                    mod_block(mod_next[0], wbuf[2], ps[2], load=False); mod_next[0] += 1
                    if mod_next[0] < 24:
                        mod_load(mod_next[0], wbuf[2])

            for f in inproj_items(0):
                f()
            for h in range(8):
                qt = QT[h % 2]; kt = KT[h % 2]; vt = VV[h % 2]
                pending = inproj_items(h + 1) if h < 7 else []
                gstep = 0
                for qc in range(2):
                    steps = []
                    for j in (3, 2, 1, 0):
                        kb = 4 * qc + j
                        steps.append((1024 + kb * 128, 8 + kb, j, False))
                    for kb in range(4 * qc - 1, -1, -1):
                        steps.append((1024 + kb * 128, 8 + kb, None, False))
                    for kb in range(7, -1, -1):
                        steps.append((kb * 128, kb, None, True))
                    n = len(steps)
                    k.op(DVE, lambda: nc.vector.memset(Rb.ap[:], 0.0), writes=[Rb])
                    qap = qt.ap[:, qc * 512:(qc + 1) * 512]
                    oT = ps[7]
                    zsrc = [None] * n
                    mod_item()
                    for it in range(n + 2):
                        gstep += 1
                        if pending and (gstep == 1 or (gstep >= 6 and gstep % 2 == 0)):
                            pending.pop(0)()
                        if it < n:
                            i = it
                            kcol, vti, dj, isprev = steps[i]
                            Z = ps[3 + i % 2]; E = Eb[i % 2]; L = Lb[i % 2]
                            k.mmg(Z, Z.ap[:], [(kt.ap[:, kcol:kcol + 128], qap)], reads=[kt, qt])
                            bias_ap = flg.ap[:, 0:1] if isprev else ZC
                            if dj is not None:
                                Zm = Zmb[i % 2]
                                k.op(DVE, lambda: nc.vector.tensor_tensor(out=Zm.ap[:], in0=Z.ap[:], in1=NEG.ap[:, dj, :], op=ALU.add), reads=[Z, NEG], writes=[Zm])
                                zs = Zm
                            else:
                                zs = Z
                            zsrc[i] = zs
                            k.op(ACT, lambda: nc.scalar.activation(out=E.ap[:], in_=zs.ap[:], func=AF.Exp, bias=bias_ap, scale=1.0), reads=[zs, flg, small], writes=[E])
                            k.op(ACT, lambda: nc.scalar.activation(out=L.ap[:], in_=E.ap[:], func=AF.Ln, bias=1.0, scale=1.0), reads=[E], writes=[L])
                        if 0 <= it - 1 < n:
                            i = it - 1
                            kcol, vti, dj, isprev = steps[i]
                            E = Eb[i % 2]; L = Lb[i % 2]; A = Ab[i % 2]; T2t = T2[i % 2]
                            zs = zsrc[i]
                            bias_ap = flg.ap[:, 0:1] if isprev else ZC
                            k.mmg(ps[5], ps[5].ap[:], [(tri_b.ap[:], L.ap[:])], reads=[tri_b, L])
                            if i < n - 1:
                                k.mmg(ps[6], ps[6].ap[:], [(ones_b.ap[:], L.ap[:])], reads=[ones_b, L])
                            k.op(DVE, lambda: nc.vector.tensor_tensor(out=T1.ap[:], in0=zs.ap[:], in1=Rb.ap[:], op=ALU.subtract), reads=[zs, Rb], writes=[T1])
                            k.op(DVE, lambda: nc.vector.tensor_tensor(out=T2t.ap[:], in0=T1.ap[:], in1=ps[5].ap[:], op=ALU.subtract), reads=[T1, ps[5]], writes=[T2t])
                            if i < n - 1:
                                k.op(DVE, lambda: nc.vector.tensor_tensor(out=Rb.ap[:], in0=Rb.ap[:], in1=ps[6].ap[:], op=ALU.add), reads=[Rb, ps[6]], writes=[Rb])
                            k.op(ACT, lambda: nc.scalar.activation(out=A.ap[:], in_=T2t.ap[:], func=AF.Exp, bias=bias_ap, scale=1.0), reads=[T2t, flg, small], writes=[A])
                        if it - 2 >= 0:
                            i = it - 2
                            kcol, vti, dj, isprev = steps[i]
                            A = Ab[i % 2]
                            k.deps(PE, [vt, A], [oT])
                            ins = nc.tensor.matmul(out=oT.ap[:], lhsT=vt.ap[:, vti, :], rhs=A.ap[:], start=(i == 0), stop=(i == n - 1))
                            PE.n += 1; ins.then_inc(PE.sem, 1); tok = (PE.sem, PE.n, PE); k.finish(tok, [vt, A], [oT])
                    k.op(ACT, lambda: nc.scalar.copy(out=concatT.ap[:, h, qc * 512:(qc + 1) * 512], in_=oT.ap[:]), reads=[oT], writes=[concatT])
                while pending:
                    pending.pop(0)()
            while mod_next[0] < 24:
                mod_item()
            k.barrier()

        with contextlib.ExitStack() as cs:
            def sbc(name, shape, dtype):
                return Tl(cs.enter_context(nc.sbuf_tensor(name, shape, dtype)), name)
            UW = 16 + NT
            U = [Tl(spare[:, i * 1040:(i + 1) * 1040], f"U{i}") for i in range(2)]
            SA = Tl(spare[:, 2080:3120], "SA"); SB_ = Tl(spare[:, 3120:4160], "SB")
            pT = Tl(spare[:, 4160:6208].bitcast(BF16).rearrange("p (c t) -> p c t", c=4), "pT")
            wpool = sbc("wpool", [128, 4, 2, 256], BF16)
            pscale = sbc("pscale", [128, 8], F32)
            invc = sbc("invc", [128, 4, 16], F32)
            tmp16 = sbc("tmp16", [128, 16], F32)
            iiD = sbc("iiD", [128, 16], I32)
            k.dma(POOL, wpool.ap[:], w_pool.rearrange("g (c p) d -> p g c d", p=128), writes=[wpool])
            k.dma(SP, pscale.ap[:], pool_scale[:, :], writes=[pscale])
            k.op(POOL, lambda: nc.gpsimd.iota(iiD.ap[:], pattern=[[1, 16]], base=1, channel_multiplier=0), writes=[iiD])
            k.op(DVE, lambda: nc.vector.tensor_copy(out=tmp16.ap[:], in_=iiD.ap[:]), reads=[iiD], writes=[tmp16])
            k.op(DVE, lambda: nc.vector.tensor_scalar(out=small.ap[:, 4:5], in0=flg.ap[:, 1:2], scalar1=1024.0, scalar2=None, op0=ALU.mult), reads=[flg], writes=[small])
            k.op(DVE, lambda: nc.vector.tensor_scalar(out=tmp16.ap[:], in0=tmp16.ap[:], scalar1=small.ap[:, 4:5], scalar2=None, op0=ALU.add), reads=[small, tmp16], writes=[tmp16])
            for g in range(4):
                k.op(DVE, lambda: nc.vector.tensor_scalar(out=invc.ap[:, g, :], in0=tmp16.ap[:], scalar1=float(2 ** (g + 1)), scalar2=None, op0=ALU.min), reads=[tmp16], writes=[invc])
            k.op(DVE, lambda: nc.vector.reciprocal(out=invc.ap[:], in_=invc.ap[:]), reads=[invc], writes=[invc])
            for g in range(4):
                w = 2 ** (g + 1)
                wb = wbuf[g % 2]
                k.dma(POOL, wb.ap[:, :, 0:256], w_in_v[:, :, 3072 + 256 * g:3072 + 256 * (g + 1)], writes=[wb])
                for cc in range(2):
                    Ut = U[cc]
                    pst = ps[2]
                    k.mmg(pst, pst.ap[:, 0:128], [(wb.ap[:, kc, cc * 128:(cc + 1) * 128], hT.ap[:, kc, 896:1024]) for kc in range(16)], reads=[wb, hT])
                    k.op(DVE, lambda: nc.vector.tensor_scalar(out=Ut.ap[:, 0:16], in0=pst.ap[:, 112:128], scalar1=flg.ap[:, 1:2], scalar2=None, op0=ALU.mult), reads=[pst, flg], writes=[Ut])
                    for n_ in range(2):
                        pst = ps[n_ % 2]
                        k.mmg(pst, pst.ap[:], [(wb.ap[:, kc, cc * 128:(cc + 1) * 128], hT.ap[:, kc, 1024 + n_ * 512:1024 + (n_ + 1) * 512]) for kc in range(16)], reads=[wb, hT])
                        k.op(ACT, lambda: nc.scalar.copy(out=Ut.ap[:, 16 + n_ * 512:16 + (n_ + 1) * 512], in_=pst.ap[:]), reads=[pst], writes=[Ut])
                    cur = Ut
                    st = 1
                    bufs = [SA, SB_]
                    bi = 0
                    while st < w:
                        nxt = bufs[bi]; bi ^= 1
                        k.op(DVE, lambda: nc.vector.tensor_tensor(out=nxt.ap[:, st:UW], in0=cur.ap[:, st:UW], in1=cur.ap[:, 0:UW - st], op=ALU.add), reads=[cur], writes=[nxt])
                        cur = nxt
                        st *= 2
                    pc = 2 * (g % 2) + cc
                    k.op(DVE, lambda: nc.vector.scalar_tensor_tensor(out=pT.ap[:, pc, :], in0=cur.ap[:, 16:UW], scalar=1.0 / w, in1=Ut.ap[:, 16:UW], op0=ALU.mult, op1=ALU.subtract), reads=[cur, Ut], writes=[pT])
                    k.op(DVE, lambda: nc.vector.tensor_tensor(out=tmp16.ap[:], in0=cur.ap[:, 16:32], in1=invc.ap[:, g, :], op=ALU.mult), reads=[cur, invc], writes=[tmp16])
                    k.op(DVE, lambda: nc.vector.tensor_tensor(out=pT.ap[:, pc, 0:16], in0=tmp16.ap[:], in1=Ut.ap[:, 16:32], op=ALU.subtract), reads=[tmp16, Ut], writes=[pT])
                for dc in range(2):
                    for n_ in range(2):
                        pst = ps[3 + n_]
                        k.mmg(pst, pst.ap[:], [(wpool.ap[:, g, cc, dc * 128:(dc + 1) * 128], pT.ap[:, 2 * (g % 2) + cc, n_ * 512:(n_ + 1) * 512]) for cc in range(2)], reads=[wpool, pT])
                        k.op(ACT, lambda: nc.scalar.activation(out=concatT.ap[:, 8 + 2 * g + dc, n_ * 512:(n_ + 1) * 512], in_=pst.ap[:], func=AF.Identity, scale=pscale.ap[:, 2 * g + dc:2 * g + dc + 1]), reads=[pst, pscale], writes=[concatT])
            k.barrier()

        x1 = [Tl(big[:, i * 2048:(i + 1) * 2048], f"x1_{i}") for i in range(8)]
        h2b = Tl(big[:, 16384:24576].bitcast(BF16).rearrange("p (i d) -> p i d", i=8), "h2b")
        w_o_v = w_o.rearrange("(c p) f -> p c f", p=128)
        with contextlib.ExitStack() as cs:
            g1b = Tl(cs.enter_context(nc.sbuf_tensor("g1b", [128, D], F32)), "g1b")
            tmpE = [Tl(cs.enter_context(nc.sbuf_tensor(f"tmpE{i}", [128, 512], F32)), f"tmpE{i}") for i in range(2)]
            k.dma(SP, g1b.ap[:], bcast_rows(mod_d.tensor, 2 * D, D), reads=[mod_dT], writes=[g1b])
            for i in range(8):
                k.dma(SP, x1[i].ap, x_own[i * 128:(i + 1) * 128, :], writes=[x1[i]])
            cnt = 0
            for nb in range(4):
                wb = wbuf[(nb + 2) % 3]
                k.dma(POOL, wb.ap[:], w_o_v[:, :, nb * 512:(nb + 1) * 512], writes=[wb])
                for i in range(8):
                    pst = ps[cnt % 2]; tm = tmpE[cnt % 2]; cnt += 1
                    k.mmg(pst, pst.ap[:], [(concatT.ap[:, kc, i * 128:(i + 1) * 128], wb.ap[:, kc, :]) for kc in range(16)], reads=[concatT, wb])
                    k.op(DVE, lambda: nc.vector.tensor_tensor(out=tm.ap[:], in0=pst.ap[:], in1=g1b.ap[:, nb * 512:(nb + 1) * 512], op=ALU.mult), reads=[pst, g1b], writes=[tm])
                    k.op(DVE, lambda: nc.vector.tensor_tensor(out=x1[i].ap[:, nb * 512:(nb + 1) * 512], in0=x1[i].ap[:, nb * 512:(nb + 1) * 512], in1=tm.ap[:], op=ALU.add), reads=[tm, x1[i]], writes=[x1[i]])
            k.barrier()
        concat_cm.__exit__(None, None, None)

        selb = Tl(sb("selb", [128, 8, NE], BF16), "selb")
        Pm = Tl(sb("Pm", [128, 8, NE], F32), "Pm")
        Gall = Tl(sb("Gall", [128, 8, NE], F32), "Gall")
        GHL = Tl(sb("GHL", [128, 8, NE, 2], BF16), "GHL")
        with contextlib.ExitStack() as cs:
            def sbc(name, shape, dtype):
                return Tl(cs.enter_context(nc.sbuf_tensor(name, shape, dtype)), name)
            A2b = sbc("A2b", [128, D], F32); B2b = sbc("B2b", [128, D], F32)
            n2b = sbc("n2b", [128, D], F32)
            h2fs = [sbc("h2f", [128, D], F32), n2b]
            h2Ts = [sbc(f"h2T{i}", [128, 16, 128], F32) for i in range(2)]
            wr = sbc("wr", [128, 16, NE], F32)
            brb = sbc("brb", [128, NE], F32)
            ssF = [sbc(f"ssF{i}", [128, 4], F32) for i in range(2)]
            lg = sbc("lg", [128, NE], F32); m8 = sbc("m8", [128, 8], F32); nm = sbc("nm", [128, 2], F32)
            selF = sbc("selF", [128, NE], F32); ex = sbc("ex", [128, NE], F32); gsum = sbc("gsum", [128, 2], F32)
            ghi = sbc("ghi", [128, NE], F32)
            psr = [ps[4], ps[6]]
            k.dma(SP, A2b.ap[:], bcast_rows(mod_d.tensor, 4 * D, D), reads=[mod_dT], writes=[A2b])
            k.dma(SP, B2b.ap[:], bcast_rows(mod_d.tensor, 3 * D, D), reads=[mod_dT], writes=[B2b])
            k.dma(SP, n2b.ap[:], bcast_rows(norm2_w.tensor, 0, D), writes=[n2b])
            k.dma(SP, wr.ap[:], w_router.rearrange("(c p) e -> p c e", p=128), writes=[wr])
            k.dma(SP, brb.ap[:], bcast_rows(b_router.tensor, 0, NE), writes=[brb])
            k.op(DVE, lambda: nc.vector.scalar_tensor_tensor(out=A2b.ap[:], in0=A2b.ap[:], scalar=1.0, in1=n2b.ap[:], op0=ALU.add, op1=ALU.mult), reads=[A2b, n2b], writes=[A2b])

            def front(i):
                ss = ssF[i % 2]; h2f = h2fs[i % 2]; h2T = h2Ts[i % 2]
                rms_rstd(x1[i], x1[i].ap, h2b.ap[:, i, :], ss, 0, D)
                k.dma(SP, x1_d[i * 128:(i + 1) * 128, :], x1[i].ap, reads=[x1[i]], writes=[x1_dT])
                k.op(DVE, lambda: nc.vector.scalar_tensor_tensor(out=h2f.ap[:], in0=x1[i].ap, scalar=ss.ap[:, 2:3], in1=A2b.ap[:], op0=ALU.mult, op1=ALU.mult), reads=[x1[i], ss, A2b], writes=[h2f])
                k.op(DVE, lambda: nc.vector.tensor_tensor(out=h2f.ap[:], in0=h2f.ap[:], in1=B2b.ap[:], op=ALU.add), reads=[h2f, B2b], writes=[h2f])
                k.op(ACT, lambda: nc.scalar.copy(out=h2b.ap[:, i, :], in_=h2f.ap[:]), reads=[h2f], writes=[h2b])
                for g in range(4):
                    pst = ps[g]
                    k.deps(PE, [h2f, ident_f], [pst])
                    ins = None
                    for j in range(4):
                        c = 4 * g + j
                        ins = nc.tensor.transpose(out=pst.ap[:, j * 128:(j + 1) * 128], in_=h2f.ap[:, c * 128:(c + 1) * 128], identity=ident_f.ap[:])
                    PE.n += 1; ins.then_inc(PE.sem, 1); tok = (PE.sem, PE.n, PE); k.finish(tok, [h2f, ident_f], [pst])
                    if g % 2 == 0:
                        k.op(ACT, lambda: nc.scalar.copy(out=h2T.ap[:, 4 * g:4 * g + 4, :], in_=pst.ap[:].rearrange("p (c t) -> p c t", c=4)), reads=[pst], writes=[h2T])
                    else:
                        k.op(DVE, lambda: nc.vector.tensor_copy(out=h2T.ap[:, 4 * g:4 * g + 4, :], in_=pst.ap[:].rearrange("p (c t) -> p c t", c=4)), reads=[pst], writes=[h2T])
                pr = psr[i % 2]
                k.mmg(pr, pr.ap[:, 0:NE], [(h2T.ap[:, kc, :], wr.ap[:, kc, :]) for kc in range(16)], reads=[h2T, wr])

            def back(i):
                pr = psr[i % 2]
                k.op(DVE, lambda: nc.vector.tensor_tensor(out=lg.ap[:], in0=pr.ap[:, 0:NE], in1=brb.ap[:], op=ALU.add), reads=[pr, brb], writes=[lg])
                k.op(DVE, lambda: nc.vector.max(out=m8.ap[:], in_=lg.ap[:]), reads=[lg], writes=[m8])
                k.op(DVE, lambda: nc.vector.tensor_scalar(out=selF.ap[:], in0=lg.ap[:], scalar1=m8.ap[:, 3:4], scalar2=None, op0=ALU.is_ge), reads=[lg, m8], writes=[selF])
                k.op(DVE, lambda: nc.vector.tensor_scalar(out=nm.ap[:, 0:1], in0=m8.ap[:, 0:1], scalar1=-1.0, scalar2=None, op0=ALU.mult), reads=[m8], writes=[nm])
                k.op(ACT, lambda: nc.scalar.activation(out=ex.ap[:], in_=lg.ap[:], func=AF.Exp, bias=nm.ap[:, 0:1], scale=1.0), reads=[lg, nm], writes=[ex])
                k.op(DVE, lambda: nc.vector.tensor_tensor(out=ex.ap[:], in0=ex.ap[:], in1=selF.ap[:], op=ALU.mult), reads=[ex, selF], writes=[ex])
                k.op(DVE, lambda: nc.vector.reduce_sum(out=gsum.ap[:, 0:1], in_=ex.ap[:], axis=mybir.AxisListType.X), reads=[ex], writes=[gsum])
                k.op(DVE, lambda: nc.vector.reciprocal(out=gsum.ap[:, 1:2], in_=gsum.ap[:, 0:1]), reads=[gsum], writes=[gsum])
                k.op(DVE, lambda: nc.vector.tensor_scalar(out=Gall.ap[:, i, :], in0=ex.ap[:], scalar1=gsum.ap[:, 1:2], scalar2=None, op0=ALU.mult), reads=[ex, gsum], writes=[Gall])
                k.op(DVE, lambda: nc.vector.tensor_copy(out=selb.ap[:, i, :], in_=selF.ap[:]), reads=[selF], writes=[selb])
                k.op(DVE, lambda: nc.vector.tensor_copy(out=GHL.ap[:, i, :, 0], in_=Gall.ap[:, i, :]), reads=[Gall], writes=[GHL])
                k.op(DVE, lambda: nc.vector.tensor_copy(out=ghi.ap[:], in_=GHL.ap[:, i, :, 0]), reads=[GHL], writes=[ghi])
                k.op(DVE, lambda: nc.vector.tensor_tensor(out=ghi.ap[:], in0=Gall.ap[:, i, :], in1=ghi.ap[:], op=ALU.subtract), reads=[Gall, ghi], writes=[ghi])
                k.op(DVE, lambda: nc.vector.tensor_copy(out=GHL.ap[:, i, :, 1], in_=ghi.ap[:]), reads=[ghi], writes=[GHL])
                ops_ = [(ones_b.ap[:], selb.ap[:, i2, :]) for i2 in range(i)] + [(strict_b.ap[:], selb.ap[:, i, :])]
                k.mmg(ps[5], ps[5].ap[:, 0:NE], ops_, reads=[ones_b, strict_b, selb])
                k.op(DVE, lambda: nc.vector.scalar_tensor_tensor(out=Pm.ap[:, i, :], in0=ps[5].ap[:, 0:NE], scalar=1.0, in1=selF.ap[:], op0=ALU.add, op1=ALU.mult), reads=[ps[5], selF], writes=[Pm])
                k.op(DVE, lambda: nc.vector.tensor_scalar(out=Pm.ap[:, i, :], in0=Pm.ap[:, i, :], scalar1=-1.0, scalar2=None, op0=ALU.add), reads=[Pm], writes=[Pm])

            front(0)
            for i in range(8):
                if i + 1 < 8:
                    front(i + 1)
                back(i)
            k.barrier()

        yacc = x1
        for i in range(8):
            k.op(DVE, lambda: nc.vector.memset(yacc[i].ap, 0.0), writes=[yacc[i]])

        with contextlib.ExitStack() as cs:
            def sbc(name, shape, dtype):
                return Tl(cs.enter_context(nc.sbuf_tensor(name, shape, dtype)), name)
            binT = sbc("binT", [128, NE * 32], F32)
            Pe = [sbc("Pe0", [128, 8, CAP], BF16)] * 2
            PT = sbc("PT", [128, 3, NT], BF16)
            XT = [sbc("XT0", [128, 16, CAP], BF16)] * 2
            HT = sbc("HT", [128, 16, CAP], BF16)
            Ygs = [sbc(f"Yg{i}", [128, 3, 512], BF16) for i in range(2)]
            gs = sbc("gs", [128, 4], F32)
            gcb = [sbc(f"gc{i}", [128, CAP], F32) for i in range(2)]
            sgb = [sbc("sg0", [128, CAP], F32)] * 2
            lcb = [sbc("lc0", [128, CAP], F32)] * 2
            stage = gcb[0]
            for r in range(8):
                k.dma(SP, stage.ap[:, 0:128], b_exp_in[r * 128:(r + 1) * 128, :], writes=[stage])
                k.deps(PE, [stage, ident_f], [ps[0]])
                ins = nc.tensor.transpose(out=ps[0].ap[:, 0:128], in_=stage.ap[:, 0:128], identity=ident_f.ap[:])
                PE.n += 1; ins.then_inc(PE.sem, 1); tok = (PE.sem, PE.n, PE); k.finish(tok, [stage, ident_f], [ps[0]])
                k.op(DVE, lambda: nc.vector.tensor_copy(out=binT.ap[:, r * 128:(r + 1) * 128], in_=ps[0].ap[:, 0:128]), reads=[ps[0]], writes=[binT])
            psb = Tl(ps[7].ap[:].bitcast(BF16), "psbf")
            SC = [(0, 128), (128, 128), (256, CAP - 256)]
            wq = []
            wcount = [0]

            def load_block(src_ap):
                wb = wbuf[wcount[0] % 3]; wcount[0] += 1
                k.dma(POOL, wb.ap[:], src_ap, writes=[wb])
                return wb

            w_in_e = w_exp_in.rearrange("e (c p) f -> e p c f", p=128)
            w_out_e = w_exp_out.rearrange("e (c p) f -> e p c f", p=128)

            def block_list(e):
                bl = []
                for j in range(4):
                    bl.append(("g", j, w_in_e[e, :, :, j * 512:(j + 1) * 512]))
                    bl.append(("l", j, w_in_e[e, :, :, D + j * 512:D + (j + 1) * 512]))
                for j in range(4):
                    bl.append(("o", j, w_out_e[e, :, :, j * 512:(j + 1) * 512]))
                return bl

            allblocks = []
            for e in range(N_EXPERTS_RUN):
                allblocks += [(e,) + b for b in block_list(e)]
            PREF = 2
            loaded = []
            for bi in range(min(PREF, len(allblocks))):
                loaded.append(load_block(allblocks[bi][3]))
            nloaded = [len(loaded)]

            def next_block(bi):
                nb_ = bi + PREF
                if nb_ < len(allblocks) and nloaded[0] <= nb_:
                    loaded.append(load_block(allblocks[nb_][3])); nloaded[0] += 1
                return loaded[bi]

            bi = 0
            upc = 0
            for e in range(N_EXPERTS_RUN):
                pe_t = Pe[e % 2]; xt_t = XT[e % 2]
                for i in range(8):
                    k.op(DVE, lambda: nc.vector.tensor_scalar(out=pe_t.ap[:, i, :], in0=iota_c.ap[:], scalar1=Pm.ap[:, i, e:e + 1], scalar2=None, op0=ALU.is_equal), reads=[iota_c, Pm], writes=[pe_t])
                for sc, (s0, sn) in enumerate(SC):
                    k.deps(PE, [pe_t, ident_b], [ps[7]])
                    ins = None
                    for i in range(8):
                        ins = nc.tensor.transpose(out=psb.ap[0:sn, i * 128:(i + 1) * 128], in_=pe_t.ap[:, i, s0:s0 + sn], identity=ident_b.ap[:])
                    PE.n += 1; ins.then_inc(PE.sem, 1); tok = (PE.sem, PE.n, PE); k.finish(tok, [pe_t, ident_b], [ps[7]])
                    k.op(ACT, lambda: nc.scalar.copy(out=PT.ap[0:sn, sc, :], in_=psb.ap[0:sn, :]), reads=[ps[7]], writes=[PT])
                    k.mmg(ps[6], ps[6].ap[0:sn, 0:2], [(pe_t.ap[:, i, s0:s0 + sn], GHL.ap[:, i, e, :]) for i in range(8)], reads=[pe_t, GHL])
                    k.op(DVE, lambda: nc.vector.reduce_sum(out=gs.ap[0:sn, sc:sc + 1], in_=ps[6].ap[0:sn, 0:2], axis=mybir.AxisListType.X), reads=[ps[6]], writes=[gs])
                for kc in range(16):
                    pst = ps[kc % 2]
                    k.mmg(pst, pst.ap[:, 0:CAP], [(h2b.ap[:, i, kc * 128:(kc + 1) * 128], pe_t.ap[:, i, :]) for i in range(8)], reads=[h2b, pe_t])
                    if kc % 2 == 0:
                        k.op(ACT, lambda: nc.scalar.copy(out=xt_t.ap[:, kc, :], in_=pst.ap[:, 0:CAP]), reads=[pst], writes=[xt_t])
                    else:
                        k.op(DVE, lambda: nc.vector.tensor_copy(out=xt_t.ap[:, kc, :], in_=pst.ap[:, 0:CAP]), reads=[pst], writes=[xt_t])
                for j in range(4):
                    wg = next_block(bi); bi += 1
                    for c in range(4):
                        fc = 4 * j + c
                        pst = ps[2 + upc % 2]; gc = gcb[upc % 2]; sg = sgb[upc % 2]; upc += 1
                        k.mmg(pst, pst.ap[:, 0:CAP], [(wg.ap[:, kc, c * 128:(c + 1) * 128], xt_t.ap[:, kc, :]) for kc in range(16)], reads=[wg, xt_t])
                        bcol = e * 32 + fc
                        k.op(DVE, lambda: nc.vector.tensor_scalar(out=gc.ap[:], in0=pst.ap[:, 0:CAP], scalar1=binT.ap[:, bcol:bcol + 1], scalar2=7.0, op0=ALU.add, op1=ALU.min), reads=[pst, binT], writes=[gc])
                        k.op(ACT, lambda: nc.scalar.activation(out=sg.ap[:], in_=gc.ap[:], func=AF.Sigmoid, scale=1.702), reads=[gc], writes=[sg])
                        k.op(DVE, lambda: nc.vector.tensor_tensor(out=HT.ap[:, fc, :], in0=gc.ap[:], in1=sg.ap[:], op=ALU.mult), reads=[gc, sg], writes=[HT])
                    wl = next_block(bi); bi += 1
                    for c in range(4):
                        fc = 4 * j + c
                        pst = ps[2 + upc % 2]; lc = lcb[upc % 2]; upc += 1
                        k.mmg(pst, pst.ap[:, 0:CAP], [(wl.ap[:, kc, c * 128:(c + 1) * 128], xt_t.ap[:, kc, :]) for kc in range(16)], reads=[wl, xt_t])
                        bcol = e * 32 + 16 + fc
                        k.op(DVE, lambda: nc.vector.tensor_scalar(out=lc.ap[:], in0=pst.ap[:, 0:CAP], scalar1=binT.ap[:, bcol:bcol + 1], scalar2=-7.0, op0=ALU.add, op1=ALU.max), reads=[pst, binT], writes=[lc])
                        k.op(DVE, lambda: nc.vector.tensor_scalar(out=lc.ap[:], in0=lc.ap[:], scalar1=7.0, scalar2=1.0, op0=ALU.min, op1=ALU.add), reads=[lc], writes=[lc])
                        k.op(DVE, lambda: nc.vector.tensor_tensor(out=HT.ap[:, fc, :], in0=HT.ap[:, fc, :], in1=lc.ap[:], op=ALU.mult), reads=[HT, lc], writes=[HT])
                def down(j):
                    wo_ = next_block(bi_box[0]); bi_box[0] += 1
                    Yg = Ygs[j % 2]
                    for sc, (s0, sn) in enumerate(SC):
                        pst = ps[4 + (3 * j + sc) % 2]
                        k.mmg(pst, pst.ap[0:sn, :], [(HT.ap[:, fcc, s0:s0 + sn], wo_.ap[:, fcc, :]) for fcc in range(16)], reads=[HT, wo_])
                        k.op(ACT, lambda: nc.scalar.activation(out=Yg.ap[0:sn, sc, :], in_=pst.ap[0:sn, :], func=AF.Identity, scale=gs.ap[0:sn, sc:sc + 1]), reads=[pst, gs], writes=[Yg])

                def scatter(j):
                    Yg = Ygs[j % 2]
                    for i in range(8):
                        pst = ps[i % 2]
                        k.mmg(pst, pst.ap[:], [(PT.ap[0:sn, sc, i * 128:(i + 1) * 128], Yg.ap[0:sn, sc, :]) for sc, (s0, sn) in enumerate(SC)], reads=[PT, Yg])
                        k.op(DVE, lambda: nc.vector.tensor_tensor(out=yacc[i].ap[:, j * 512:(j + 1) * 512], in0=yacc[i].ap[:, j * 512:(j + 1) * 512], in1=pst.ap[:], op=ALU.add), reads=[pst, yacc[i]], writes=[yacc[i]])
                bi_box = [bi]
                down(0)
                for j in range(4):
                    if j + 1 < 4:
                        down(j + 1)
                    scatter(j)
                bi = bi_box[0]
            k.barrier()

        with contextlib.ExitStack() as cs:
            def sbc(name, shape, dtype):
                return Tl(cs.enter_context(nc.sbuf_tensor(name, shape, dtype)), name)
            g2b = sbc("g2b", [128, D], F32)
            bo = sbc("bo", [NE, D], F32)
            xr = [sbc(f"xr{i}", [128, D], F32) for i in range(2)]
            k.dma(SP, g2b.ap[:], bcast_rows(mod_d.tensor, 5 * D, D), reads=[mod_dT], writes=[g2b])
            k.dma(SP, bo.ap[:], b_exp_out[:, :], writes=[bo])
            GT = sbc("GT", [NE, NT], F32)
            for i in range(8):
                k.deps(PE, [Gall, ident_f], [ps[6]])
                ins = nc.tensor.transpose(out=ps[6].ap[0:NE, 0:128], in_=Gall.ap[:, i, :], identity=ident_f.ap[:])
                PE.n += 1; ins.then_inc(PE.sem, 1); tok = (PE.sem, PE.n, PE); k.finish(tok, [Gall, ident_f], [ps[6]])
                k.op(ACT, lambda: nc.scalar.copy(out=GT.ap[:, i * 128:(i + 1) * 128], in_=ps[6].ap[0:NE, 0:128]), reads=[ps[6]], writes=[GT])
            cc_ = 0
            for i in range(8):
                xt = xr[i % 2]
                k.dma(SP, xt.ap[:], x1_d[i * 128:(i + 1) * 128, :], reads=[x1_dT], writes=[xt])
                for nb in range(4):
                    pst = ps[cc_ % 2]; cc_ += 1
                    k.mmg(pst, pst.ap[:], [(GT.ap[:, i * 128:(i + 1) * 128], bo.ap[:, nb * 512:(nb + 1) * 512])], reads=[GT, bo])
                    sl = slice(nb * 512, (nb + 1) * 512)
                    k.op(DVE, lambda: nc.vector.tensor_tensor(out=yacc[i].ap[:, sl], in0=yacc[i].ap[:, sl], in1=pst.ap[:], op=ALU.add), reads=[pst, yacc[i]], writes=[yacc[i]])
                k.op(DVE, lambda: nc.vector.tensor_tensor(out=yacc[i].ap, in0=yacc[i].ap, in1=g2b.ap[:], op=ALU.mult), reads=[yacc[i], g2b], writes=[yacc[i]])
                k.op(DVE, lambda: nc.vector.tensor_tensor(out=xt.ap[:], in0=xt.ap[:], in1=yacc[i].ap, op=ALU.add), reads=[yacc[i], xt], writes=[xt])
                k.dma(SP, out[i * 128:(i + 1) * 128, :], xt.ap[:], reads=[xt], writes=[outT])
            k.wait(SP, (outT.dsem, outT.dcnt, None))
            if DEBUG:
                k.wait(SP, (x1_dT.dsem, x1_dT.dcnt, None))

    return nc


def make_in_maps(inputs, ne_w=NE):
    f = lambda a: np.ascontiguousarray(np.asarray(a, dtype=np.float32))
    x = f(inputs["x"]); c = f(inputs["c"])
    shared = {
        "norm1_w": f(np.asarray(inputs["norm1_w"])[0].reshape(16, 128).T),
        "norm2_w": f(np.asarray(inputs["norm2_w"])[0]),
        "w_ada": f(np.asarray(inputs["w_ada"])[0]),
        "b_ada": f(np.asarray(inputs["b_ada"])[0]),
        "w_in": f(np.asarray(inputs["w_in"])[0]),
        "qk_w": f(np.stack([np.asarray(inputs["q_norm_w"])[0], np.asarray(inputs["k_norm_w"])[0]], axis=1)),
        "w_pool": f(np.asarray(inputs["w_pool"])[0]),
        "pool_scale": f(np.asarray(inputs["pool_scale"])[0].reshape(8, 128).T),
        "w_o": f(np.asarray(inputs["w_o"])[0]),
        "w_router": f(np.asarray(inputs["w_router"])[0]),
        "b_router": f(np.asarray(inputs["b_router"])[0]),
        "w_exp_in": f(np.asarray(inputs["w_exp_in"])[0][:ne_w]),
        "b_exp_in": f(np.asarray(inputs["b_exp_in"])[0].reshape(NE * 32, 128)),
        "w_exp_out": f(np.asarray(inputs["w_exp_out"])[0][:ne_w]),
        "b_exp_out": f(np.asarray(inputs["b_exp_out"])[0]),
    }
    in_maps = []
    for core in range(8):
        b, half = core // 2, core % 2
        fl = np.zeros((128, 2), np.float32)
        fl[:, 0] = 0.0 if half == 1 else NEGB
        fl[:, 1] = 1.0 if half == 1 else 0.0
        m = dict(shared)
        m["x_own"] = f(x[b, half * NT:(half + 1) * NT])
        m["x_prev"] = f(x[b, 0:NT])
        m["c_own"] = f(c[b].reshape(16, 128).T)
        m["flags"] = fl
        in_maps.append(m)
    return in_maps


_NC_CACHE = {}


def kernel(**inputs):
    in_maps = make_in_maps(inputs)
    if "nc" not in _NC_CACHE:
        _NC_CACHE["nc"] = build_nc()
    nc = _NC_CACHE["nc"]
    res = run_bass_kernel_spmd(nc, in_maps, core_ids=list(range(8)))
    outp = np.zeros((4, 2048, D), np.float32)
    for core in range(8):
        b, half = core // 2, core % 2
        outp[b, half * NT:(half + 1) * NT] = res.results[core]["out"]
    return outp
```
